# Optimizing a Trainium2 kernel written in Bass

```python
import jax, jax.numpy as jnp
from jax import lax
import numpy as np

D_MODEL = 2048
BATCH = 4
SEQ = 4096
DEPTH = 1

CHUNK = 64
LEFT_CHUNKS = 8
BAND = LEFT_CHUNKS + 1
D_MIX = D_MODEL
D_ATTN = D_MIX // 2
D_CONV = D_MIX - D_ATTN
HEAD_DIM = 64
N_HEADS = D_ATTN // HEAD_DIM
REL_CLIP = 256
N_REL = 2 * REL_CLIP + 1
CONV_WIDTH = 3
D_IN_PROJ = 3 * D_ATTN + 3 * D_CONV

N_GROUPS = 4
EXPERTS_PER_GROUP = 8
N_EXPERTS = N_GROUPS * EXPERTS_PER_GROUP
TOP_K_INNER = 2
D_EXPERT = 512
MOE_BLOCK = 128

EPS = 1e-6
NEG_INF = -1e30

kernel_name = "hymba_chunk_attn_shortconv_hiermoe"


def rmsnorm(x, g):
    xf = x.astype(jnp.float32)
    y = xf * lax.rsqrt(jnp.mean(xf * xf, axis=-1, keepdims=True) + EPS)
    return (y * g.astype(jnp.float32)).astype(x.dtype)


def chunk_attention(q, k, v, rel_bias):
    b, s = q.shape[0], q.shape[1]
    nc = s // CHUNK
    qc = q.reshape(b, nc, CHUNK, N_HEADS, HEAD_DIM)
    pad = ((0, 0), (LEFT_CHUNKS, 0), (0, 0), (0, 0), (0, 0))
    kp = jnp.pad(k.reshape(b, nc, CHUNK, N_HEADS, HEAD_DIM), pad)
    vp = jnp.pad(v.reshape(b, nc, CHUNK, N_HEADS, HEAD_DIM), pad)
    k_band = jnp.concatenate([kp[:, o:o + nc] for o in range(BAND)], axis=2)
    v_band = jnp.concatenate([vp[:, o:o + nc] for o in range(BAND)], axis=2)
    qi = jnp.arange(CHUNK)[:, None]
    kj = jnp.arange(BAND * CHUNK)[None, :]
    dist = LEFT_CHUNKS * CHUNK + qi - kj
    idx = jnp.clip(dist, -REL_CLIP, REL_CLIP) + REL_CLIP
    bias = rel_bias[:, idx].astype(jnp.float32)
    key_chunk = jnp.arange(nc)[:, None] - LEFT_CHUNKS + (jnp.arange(BAND * CHUNK) // CHUNK)[None, :]
    valid = key_chunk >= 0
    scale = HEAD_DIM ** -0.5
    scores = jnp.einsum('bnqhd,bnkhd->bnhqk', qc, k_band).astype(jnp.float32) * scale + bias
    scores = jnp.where(valid[None, :, None, None, :], scores, jnp.float32(NEG_INF))
    p = jax.nn.softmax(scores, axis=-1).astype(v.dtype)
    o = jnp.einsum('bnhqk,bnkhd->bnqhd', p, v_band)
    return o.reshape(b, s, N_HEADS * HEAD_DIM)


def short_conv(bg, cg, u, w):
    z = cg * u
    y = lax.conv_general_dilated(
        z, w[:, None, :], window_strides=(1,), padding=[(CONV_WIDTH - 1, 0)],
        dimension_numbers=('NWC', 'WIO', 'NWC'), feature_group_count=D_CONV)
    return bg * y


def hier_moe(h, w_rg, b_rg, w_re, b_re, w_gate, w_up, w_down):
    bsz, s, d = h.shape
    t = bsz * s
    xt = h.reshape(t, d)
    tok = jnp.arange(t)
    glog = (xt @ w_rg + b_rg).astype(jnp.float32)
    gprob = jax.nn.softmax(glog, axis=-1)
    g_sel = jnp.argmax(glog, axis=-1).astype(jnp.int32)
    p_g = gprob[tok, g_sel]
    elog = (xt @ w_re + b_re).astype(jnp.float32).reshape(t, N_GROUPS, EXPERTS_PER_GROUP)
    elog_g = elog[tok, g_sel]
    top_v, top_i = lax.top_k(elog_g, TOP_K_INNER)
    w_tok = p_g[:, None] * jax.nn.softmax(top_v, axis=-1)
    eid = g_sel[:, None] * EXPERTS_PER_GROUP + top_i.astype(jnp.int32)

    n = t * TOP_K_INNER
    flat_e = eid.reshape(-1)
    flat_w = w_tok.reshape(-1)
    flat_tok = (jnp.arange(n) // TOP_K_INNER).astype(jnp.int32)
    order = jnp.argsort(flat_e, stable=True)
    se, stok, sw = flat_e[order], flat_tok[order], flat_w[order]
    counts = jnp.bincount(flat_e, length=N_EXPERTS).astype(jnp.int32)
    starts = jnp.cumsum(counts) - counts
    pcounts = (counts + MOE_BLOCK - 1) // MOE_BLOCK * MOE_BLOCK
    pends = jnp.cumsum(pcounts)
    pstarts = pends - pcounts
    dest = pstarts[se] + jnp.arange(n, dtype=jnp.int32) - starts[se]
    n_blocks = -(-n // MOE_BLOCK) + N_EXPERTS
    npad = n_blocks * MOE_BLOCK
    buf_tok = jnp.full((npad,), t, dtype=jnp.int32).at[dest].set(stok)
    buf_w = jnp.zeros((npad,), jnp.float32).at[dest].set(sw)
    block_e = jnp.minimum(
        jnp.searchsorted(pends, jnp.arange(n_blocks, dtype=jnp.int32) * MOE_BLOCK, side='right'),
        N_EXPERTS - 1).astype(jnp.int32)
    xpad = jnp.concatenate([xt, jnp.zeros((1, d), xt.dtype)], axis=0)
    xb = xpad[buf_tok].reshape(n_blocks, MOE_BLOCK, d)

    def expert_block(args):
        xblk, e = args
        a = jax.nn.silu(xblk @ w_gate[e]) * (xblk @ w_up[e])
        return a @ w_down[e]

    yb = lax.map(expert_block, (xb, block_e))
    y = yb.reshape(npad, d) * buf_w[:, None].astype(xt.dtype)
    out = jnp.zeros((t + 1, d), xt.dtype).at[buf_tok].add(y)[:t]
    return out.reshape(bsz, s, d)


def setup_inputs(seed: int = 0) -> dict:
    key = jax.random.key(seed)
    ks = jax.random.split(key, 18)
    f32 = jnp.float32
    nrm = lambda k, shape, sc: (jax.random.normal(k, shape, f32) * sc).astype(f32)
    return {
        "x": nrm(ks[0], (BATCH, SEQ, D_MODEL), 1.0),
        "norm1": 1.0 + nrm(ks[1], (DEPTH, D_MODEL), 0.02),
        "w_in": nrm(ks[2], (DEPTH, D_MODEL, D_IN_PROJ), D_MODEL ** -0.5),
        "rel_bias": nrm(ks[3], (DEPTH, N_HEADS, N_REL), 0.1),
        "conv_w": nrm(ks[4], (DEPTH, CONV_WIDTH, D_CONV), CONV_WIDTH ** -0.5),
        "g_out_attn": 1.0 + nrm(ks[5], (DEPTH, D_ATTN), 0.02),
        "g_out_conv": 1.0 + nrm(ks[6], (DEPTH, D_CONV), 0.02),
        "w_out": nrm(ks[7], (DEPTH, D_MIX, D_MODEL), D_MIX ** -0.5),
        "norm2": 1.0 + nrm(ks[8], (DEPTH, D_MODEL), 0.02),
        "w_router_group": nrm(ks[9], (DEPTH, D_MODEL, N_GROUPS), D_MODEL ** -0.5),
        "b_router_group": nrm(ks[10], (DEPTH, N_GROUPS), 0.01),
        "w_router_expert": nrm(ks[11], (DEPTH, D_MODEL, N_EXPERTS), D_MODEL ** -0.5),
        "b_router_expert": nrm(ks[12], (DEPTH, N_EXPERTS), 0.01),
        "w_gate": nrm(ks[13], (DEPTH, N_EXPERTS, D_MODEL, D_EXPERT), D_MODEL ** -0.5),
        "w_up": nrm(ks[14], (DEPTH, N_EXPERTS, D_MODEL, D_EXPERT), D_MODEL ** -0.5),
        "w_down": nrm(ks[15], (DEPTH, N_EXPERTS, D_EXPERT, D_MODEL), D_EXPERT ** -0.5),
        "norm_final": 1.0 + nrm(ks[16], (D_MODEL,), 0.02),
    }


def reference(x, norm1, w_in, rel_bias, conv_w, g_out_attn, g_out_conv, w_out, norm2,
              w_router_group, b_router_group, w_router_expert, b_router_expert,
              w_gate, w_up, w_down, norm_final):
    b, s, _ = x.shape
    for l in range(DEPTH):
        h = rmsnorm(x, norm1[l])
        proj = h @ w_in[l]
        q, k, v, bg, cg, u = jnp.split(proj, 6, axis=-1)
        q = q.reshape(b, s, N_HEADS, HEAD_DIM)
        k = k.reshape(b, s, N_HEADS, HEAD_DIM)
        v = v.reshape(b, s, N_HEADS, HEAD_DIM)
        a_out = chunk_attention(q, k, v, rel_bias[l])
        c_out = short_conv(bg, cg, u, conv_w[l])
        mix = jnp.concatenate([rmsnorm(a_out, g_out_attn[l]), rmsnorm(c_out, g_out_conv[l])], axis=-1)
        x = x + mix @ w_out[l]
        h2 = rmsnorm(x, norm2[l])
        x = x + hier_moe(h2, w_router_group[l], b_router_group[l], w_router_expert[l],
                         b_router_expert[l], w_gate[l], w_up[l], w_down[l])
    return rmsnorm(x, norm_final)
```

```python
import os
from contextlib import ExitStack

import ml_dtypes
import numpy as np

import concourse.bass as bass
import concourse.mybir as mybir
from concourse.bass_utils import run_bass_kernel_spmd

F32 = mybir.dt.float32
BF = mybir.dt.bfloat16
I32 = mybir.dt.int32
ALU = mybir.AluOpType
AF = mybir.ActivationFunctionType
AX = mybir.AxisListType
bf16 = ml_dtypes.bfloat16

NCORES = 8
D = 2048
KT = 16
T = 2048
HALO = 512
TE = T + HALO
NTT = T // 128
NTE = TE // 128
DA = 1024
DC = 1024
NE = 32
DE = 512
CAP = 320
NSUB = (CAP + 127) // 128
RSZ = [min(128, CAP - 128 * i) for i in range(NSUB)]
NSLOT = NE * CAP
EPS = 1e-6
BIGT = 20000.0
VW = 66

ENGS = ("pe", "dve", "act", "pool", "sp")


class Op:
    __slots__ = ("eng", "emit", "deps", "is_dma", "sem_key", "val", "signal")

    def __init__(self, eng, emit, is_dma, sem_key=None, val=0):
        self.eng = eng
        self.emit = emit
        self.deps = []
        self.is_dma = is_dma
        self.sem_key = sem_key
        self.val = val
        self.signal = False


class Prog:
    def __init__(self, nc, stack):
        self.nc = nc
        self.pending = {e: [] for e in ENGS}
        self.cnt = {e: 0 for e in ENGS}
        self.dma_cnt = {}
        self.sems = {}
        self.waited = {e: {} for e in ENGS}
        self.last_w = {}
        self.readers = {}
        self.last_op = {e: None for e in ENGS}
        self._stack = stack
        self._old_dmas = []

    def _sem(self, name):
        if name not in self.sems:
            self.sems[name] = self._stack.enter_context(self.nc.semaphore(name))
        return self.sems[name]

    def _track(self, op, reads, writes, extra):
        deps = []
        for k in reads:
            w = self.last_w.get(k)
            if w is not None:
                deps.append(w)
        for k in writes:
            w = self.last_w.get(k)
            if w is not None:
                deps.append(w)
            deps.extend(self.readers.get(k, ()))
        deps.extend(extra)
        seen = set()
        for d in deps:
            if d is None or d is op or id(d) in seen:
                continue
            if d.eng == "pe" and op.eng == "pe" and not d.is_dma and not op.is_dma:
                continue
            seen.add(id(d))
            op.deps.append(d)
            if not d.is_dma:
                d.signal = True
        for k in reads:
            self.readers.setdefault(k, []).append(op)
        for k in writes:
            self.last_w[k] = op
            self.readers[k] = []

    def op(self, eng, emit, reads=(), writes=(), deps=()):
        o = Op(eng, emit, False)
        self._track(o, reads, writes, deps)
        self.pending[eng].append(o)
        self.last_op[eng] = o
        return o

    def dma(self, eng, emit, sem_key, reads=(), writes=(), deps=()):
        c = self.dma_cnt.get(sem_key, 0) + 16
        self.dma_cnt[sem_key] = c
        o = Op(eng, emit, True, sem_key="d_" + sem_key, val=c)
        self._track(o, reads, writes, deps)
        self.pending[eng].append(o)
        return o

    def barrier(self):
        lasts = [self.last_op[e] for e in ENGS if self.last_op[e] is not None]
        best = {}
        for o in [o for e in ENGS for o in self.pending[e] if o.is_dma] + self._old_dmas:
            if o.sem_key not in best or best[o.sem_key].val < o.val:
                best[o.sem_key] = o
        alld = lasts + list(best.values())
        for e in ENGS:
            o = Op(e, None, False)
            for d in alld:
                o.deps.append(d)
                if not d.is_dma:
                    d.signal = True
            self.pending[e].append(o)
        self.last_w = {}
        self.readers = {}

    def flush(self):
        nc = self.nc
        for e in ENGS:
            for o in self.pending[e]:
                if not o.is_dma and o.signal:
                    self.cnt[e] += 1
                    o.val = self.cnt[e]
                    o.sem_key = "e_" + e
        for e in ENGS:
            for o in self.pending[e]:
                if o.sem_key is not None:
                    self._sem(o.sem_key)
        pend = self.pending
        self.pending = {e: [] for e in ENGS}
        best = {}
        for o in [o for e in ENGS for o in pend[e] if o.is_dma] + self._old_dmas:
            if o.sem_key not in best or best[o.sem_key].val < o.val:
                best[o.sem_key] = o
        self._old_dmas = list(best.values())

        def run(e, engine):
            waited = self.waited[e]
            for o in pend[e]:
                for d in sorted(o.deps, key=lambda d_: -d_.val):
                    if waited.get(d.sem_key, 0) >= d.val:
                        continue
                    engine.wait_ge(self.sems[d.sem_key], d.val)
                    waited[d.sem_key] = d.val
                if o.emit is None:
                    continue
                inst = o.emit(engine)
                if o.is_dma:
                    inst.then_inc(self.sems[o.sem_key], 16)
                elif o.signal:
                    inst.then_inc(self.sems[o.sem_key], 1)

        with nc.Block() as block:
            if pend["pe"]:
                @block.tensor
                def _(eng):
                    run("pe", eng)
            if pend["dve"]:
                @block.vector
                def _(eng):
                    run("dve", eng)
            if pend["act"]:
                @block.scalar
                def _(eng):
                    run("act", eng)
            if pend["pool"]:
                @block.gpsimd
                def _(eng):
                    run("pool", eng)
            if pend["sp"]:
                @block.sync
                def _(eng):
                    run("sp", eng)


def build(stage=99):
    nc = bass.Bass("TRN2", target_bir_lowering=False)

    def din(name, shape, dt=F32):
        return nc.dram_tensor(name, shape, dt, kind="ExternalInput").ap()

    x_ext = din("x_ext", [TE, D])
    valid_d = din("valid", [128, NTE])
    w_in = din("w_in", [24, 128, KT * 256])
    g1T_d = din("g1T", [128, KT])
    biasT_d = din("biasT", [128, 16, 640])
    cwT_d = din("cwT", [128, 8, 3])
    gaT_d = din("gaT", [128, 8])
    gcT_d = din("gcT", [128, 8])
    w_out = din("w_out", [128, KT * D])
    g2b_d = din("g2b", [128, D])
    gfb_d = din("gfb", [128, D])
    wr_d = din("wr", [128, KT * 36])
    brb_d = din("brb", [128, 36])
    w_gate = din("w_gate", [NE, 128, KT * DE])
    w_up = din("w_up", [NE, 128, KT * DE])
    w_down = din("w_down", [NE, 128, 4 * D])
    ident_d = din("ident", [128, 128], BF)
    tri_d = din("tri", [128, 128], BF)
    ones_d = din("ones", [128, 128], BF)
    ecap_d = din("ecap", [128, NE])
    out_d = nc.dram_tensor("out", [T, D], F32, kind="ExternalOutput").ap()
    if stage < 99:
        x2s = nc.dram_tensor("x2s", [T, D], F32, kind="ExternalOutput").ap()
        dbg = nc.dram_tensor("dbg", [128, NTT, 8], F32, kind="ExternalOutput").ap()
    else:
        x2s = nc.dram_tensor("x2s", [T, D], F32).ap()
        dbg = None
    NCONV = 19
    wgb = nc.dram_tensor("wgb", [NCONV, 128, KT * DE], BF).ap()
    wub = nc.dram_tensor("wub", [NCONV, 128, KT * DE], BF).ap()
    wdb = nc.dram_tensor("wdb", [NCONV, 128, 4 * D], BF).ap()
    wob = nc.dram_tensor("wob", [128, KT * D], BF).ap()
    Xs = nc.dram_tensor("Xs", [NSLOT, D], BF).ap()
    Ys = nc.dram_tensor("Ys", [NSLOT + 128, D], BF).ap()

    def wblk(c0):
        return w_in[c0 // 256].rearrange("p (k c) -> p k c", k=KT)

    w_out_r = w_out.rearrange("p (f c) -> p f c", f=KT)
    wob_r = wob.rearrange("p (f c) -> p f c", f=KT)

    BASE = 16640
    TOP = 229376
    SMALL = TOP - 6144
    esz = {F32: 4, BF: 2, I32: 4}
    cur = [BASE]
    uid = [0]

    def at(name, shape, dt, off):
        uid[0] += 1
        n = 1
        for s in shape[1:]:
            n *= s
        assert BASE + off + n * esz[dt] <= TOP, (name, off, n * esz[dt])
        return nc.alloc_sbuf_tensor_at(f"{name}_{uid[0]}", list(shape), dt, offset=BASE + off)

    def loc(name, shape, dt, limit=SMALL):
        n = 1
        for s in shape[1:]:
            n *= s
        sz = (n * esz[dt] + 31) // 32 * 32
        off = cur[0]
        assert off + sz <= limit, (name, off, sz, limit)
        cur[0] = off + sz
        uid[0] += 1
        return nc.alloc_sbuf_tensor_at(f"{name}_{uid[0]}", list(shape), dt, offset=off)

    def set_cur(off):
        cur[0] = BASE + off

    K = 1024

    with ExitStack() as top:
        P = Prog(nc, top)

        def MM(out, lhsT, rhs, start, stop, reads, writes, skip=False):
            if skip:
                P.op("pe", lambda e: e.matmul(out, lhsT=lhsT, rhs=rhs, start=start, stop=stop,
                                              skip_group_check=True), reads, writes)
            else:
                P.op("pe", lambda e: e.matmul(out, lhsT=lhsT, rhs=rhs, start=start, stop=stop),
                     reads, writes)

        def TR(out, in_, reads, writes):
            P.op("pe", lambda e: e.transpose(out=out, in_=in_, identity=ident[:]), reads, writes)

        def ACTF(out, in_, func, reads, writes, **kw):
            P.op("act", lambda e: e.activation(out=out, in_=in_, func=func, **kw), reads, writes)

        def TS(out, in0, s1, s2, op0, op1, reads, writes, eng="dve"):
            P.op(eng, lambda e: e.tensor_scalar(out=out, in0=in0, scalar1=s1, scalar2=s2, op0=op0, op1=op1),
                 reads, writes)

        def STT(out, in0, scalar, in1, op0, op1, reads, writes, eng="dve"):
            P.op(eng, lambda e: e.scalar_tensor_tensor(out=out, in0=in0, scalar=scalar, in1=in1,
                                                       op0=op0, op1=op1), reads, writes)

        def TT(out, in0, in1, op, reads, writes, eng="dve"):
            P.op(eng, lambda e: e.tensor_tensor(out=out, in0=in0, in1=in1, op=op), reads, writes)

        def CP(eng, out, in_, reads, writes):
            if eng == "act":
                P.op("act", lambda e: e.copy(out=out, in_=in_), reads, writes)
            else:
                P.op(eng, lambda e: e.tensor_copy(out=out, in_=in_), reads, writes)

        def RED(out, in_, op, reads, writes):
            P.op("dve", lambda e: e.tensor_reduce(out=out, in_=in_, axis=AX.X, op=op), reads, writes)

        def RCP(out, in_, reads, writes):
            P.op("dve", lambda e: e.reciprocal(out=out, in_=in_), reads, writes)

        def DMA(q, out, in_, key, reads, writes):
            return P.dma(q, lambda e: e.dma_start(out=out, in_=in_), key, reads, writes)

        def rstd_chain(ss, tmp, srt, out, n, inv_n, rk, wk):
            ACTF(srt, ss, AF.Sqrt, rk, [wk + "_s"], scale=inv_n, bias=epsT[:, 0:1])
            RCP(out, srt, [wk + "_s"], [wk])

        conv_list = []
        for q_ in range(4):
            conv_list.append((wob[:, q_ * 4 * D:(q_ + 1) * 4 * D], w_out[:, q_ * 4 * D:(q_ + 1) * 4 * D]))
        for e_ in range(NCONV):
            conv_list.append((wgb[e_], w_gate[e_]))
            conv_list.append((wub[e_], w_up[e_]))
            conv_list.append((wdb[e_], w_down[e_]))
        conv_pos = [0]

        def emit_conv(n):
            for _ in range(n):
                if conv_pos[0] >= len(conv_list):
                    return
                o_, i_ = conv_list[conv_pos[0]]
                DMA("pool", o_, i_, f"cv{conv_pos[0] % 4}", [], [])
                conv_pos[0] += 1

        cur[0] = SMALL
        LIM = TOP
        ident = loc("ident", [128, 128], BF, LIM)
        tri = loc("tri", [128, 128], BF, LIM)
        ones = loc("ones", [128, 128], BF, LIM)
        g1T = loc("g1T", [128, KT], F32, LIM)
        gaT = loc("gaT", [128, 8], F32, LIM)
        gcT = loc("gcT", [128, 8], F32, LIM)
        cwT = loc("cwT", [128, 8, 3], F32, LIM)
        valid = loc("valid", [128, NTE], F32, LIM)
        ecap = loc("ecap", [128, NE], F32, LIM)
        brb = loc("brb", [128, 36], F32, LIM)
        sl_i = loc("sl_i", [128, NTT, 2], I32, LIM)
        gw = loc("gw", [128, NTT, 2], F32, LIM)
        rstd_c = loc("rstd_c", [128, NTT], F32, LIM)
        rstd_a = loc("rstd_a", [128, NTT], F32, LIM)
        ssa = loc("ssa", [128, NTT], F32, LIM)
        tmp16 = loc("tmp16", [128, NTT], F32, LIM)
        tmp16b = loc("tmp16b", [128, NTT], F32, LIM)
        cnt = loc("cnt", [128, NE], F32, LIM)
        epsT = loc("epsT", [128, 1], F32, LIM)
        st6 = [loc(f"st6{i}", [128, 4], F32, LIM) for i in range(6)]
        st = [loc(f"st{i}", [128, 4], F32, LIM) for i in range(2)]
        rden = [loc(f"rden{i}", [128, 1], F32, LIM) for i in range(2)]
        lg = loc("lg", [128, 36], F32, LIM)
        gmx = loc("gmx", [128, 4], F32, LIM)
        ohg = loc("ohg", [128, 4], F32, LIM)
        pen = loc("pen", [128, 4], F32, LIM)
        eg = loc("eg", [128, 4], F32, LIM)
        elm = loc("elm", [128, NE], F32, LIM)
        m8 = loc("m8", [128, 8], F32, LIM)
        oh = loc("oh", [128, 2, NE], F32, LIM)
        prod = loc("prod", [128, 2, NE], F32, LIM)
        Mb = loc("Mb", [128, NE], BF, LIM)
        rk = loc("rk", [128, NE], F32, LIM)
        tmpe = loc("tmpe", [128, NE], F32, LIM)
        slf = loc("slf", [128, 2], F32, LIM)
        rr = loc("rr", [128, 2], F32, LIM)
        tov = loc("tov", [128, 2], F32, LIM)
        dsl = loc("dsl", [128, 2], F32, LIM)
        gsc = loc("gsc", [128, 4], F32, LIM)

        for (t_, d_, k_) in ((ident, ident_d, "c0"), (tri, tri_d, "c1"), (ones, ones_d, "c2"), (g1T, g1T_d, "c3"),
                             (gaT, gaT_d, "c4"), (gcT, gcT_d, "c5"), (cwT, cwT_d, "c6"), (valid, valid_d, "c7"),
                             (ecap, ecap_d, "c8"), (brb, brb_d, "c9")):
            DMA("sp", t_[:], d_, k_, [], [k_])
        P.op("dve", lambda e: e.memset(cnt[:], 0.0), [], ["cnt"])
        P.op("dve", lambda e: e.memset(epsT[:], EPS), [], ["epsT"])
        P.barrier()

        hT = at("hT", [128, KT, TE], BF, 0)
        set_cur(112 * K)
        wq = loc("wq", [128, KT, 256], BF)
        wk = loc("wk", [128, KT, 256], BF)
        wv = loc("wv", [128, KT, 256], BF)

        def LOADQKV(g):
            DMA("pool", wq[:], wblk(g * 256), "wq", [], ["wq"])
            DMA("pool", wk[:], wblk(1024 + g * 256), "wk", [], ["wk"])
            DMA("pool", wv[:], wblk(2048 + g * 256), "wv", [], ["wv"])

        set_cur(137 * K)
        xin = [loc(f"xin{i}", [128, D], F32) for i in range(6)]
        xs = [loc(f"xs{i}", [128, D], BF) for i in range(3)]
        junk = loc("junk", [128, D], BF)
        with ExitStack() as ph:
            tp = [[ph.enter_context(nc.psum_tensor(f"tp{s}{h}", [128, 8, 128], BF)) for h in range(2)]
                  for s in range(2)]

            def L1(tt):
                s6 = tt % 6
                DMA("sp", xin[s6][:], x_ext[tt * 128:(tt + 1) * 128, :], f"xin{s6}", [], [f"xin{s6}"])

            def A1(tt):
                s6 = tt % 6
                ACTF(junk[:], xin[s6][:], AF.Square, [f"xin{s6}"], ["junk", f"ss{s6}"], accum_out=st6[s6][:, 0:1])

            def B1(tt):
                s6 = tt % 6
                rstd_chain(st6[s6][:, 0:1], None, st6[s6][:, 2:3], st6[s6][:, 3:4], 1, 1.0 / D, [f"ss{s6}"], f"rs{s6}")

            def C1(tt):
                s6 = tt % 6
                s3 = tt % 3
                s = tt % 2
                TS(xs[s3][:], xin[s6][:], st6[s6][:, 3:4], 0.0, ALU.mult, ALU.add, [f"xin{s6}", f"rs{s6}"],
                   [f"xs{s3}"], eng="pool")
                for h in range(2):
                    for j in range(8):
                        kt = h * 8 + j
                        TR(tp[s][h][:, j, :], xs[s3][:, kt * 128:(kt + 1) * 128], [f"xs{s3}"], [f"tp{s}{h}"])
                    TT(hT[:, h * 8:(h + 1) * 8, tt * 128:(tt + 1) * 128], tp[s][h][:],
                       g1T[:, h * 8:(h + 1) * 8].unsqueeze(2).to_broadcast([128, 8, 128]), ALU.mult,
                       [f"tp{s}{h}"], ["hT"])

            for tt in range(4):
                L1(tt)
            A1(0)
            A1(1)
            A1(2)
            B1(0)
            B1(1)
            for tt in range(NTE):
                if tt == 6:
                    LOADQKV(0)
                if tt + 4 < NTE:
                    L1(tt + 4)
                if tt + 3 < NTE:
                    A1(tt + 3)
                if tt + 2 < NTE:
                    B1(tt + 2)
                C1(tt)
            P.barrier()
            P.flush()

        aout = at("aout", [128, NTT, DA], BF, 80 * K)
        set_cur(112 * K)
        set_cur(136 * K)
        QT = loc("QT", [128, 2, T], BF)
        KTt = loc("KTt", [128, 2, TE], BF)
        Vx = loc("Vx", [128, NTE, 4, VW], BF)
        biasS = loc("biasS", [128, 4, 640], F32)
        sb = [loc(f"sb{i}", [128, 640], F32) for i in range(3)]
        PT = [loc(f"PT{i}", [128, 640], BF) for i in range(3)]
        junk2 = loc("junk2", [128, DA], BF)
        with ExitStack() as ph:
            pj = [ph.enter_context(nc.psum_tensor(f"pj{i}", [128, 512], F32)) for i in range(2)]
            sA = [ph.enter_context(nc.psum_tensor(f"sA{i}", [128, 512], F32)) for i in range(2)]
            sB = [ph.enter_context(nc.psum_tensor(f"sB{i}", [128, 512], F32)) for i in range(2)]
            oP = [ph.enter_context(nc.psum_tensor(f"oP{i}", [128, 512], F32)) for i in range(2)]
            sA3 = [sA[0], sA[1], pj[0]]
            sB3 = [sB[0], sB[1], pj[1]]
            kA3 = ["sA0", "sA1", "pj0"]
            kB3 = ["sB0", "sB1", "pj1"]
            for h4 in range(4):
                CP("dve", Vx[:, :, h4, 64], valid[:, :], [], ["Vx1"])
            pjc = [0]

            def proj_fm(w, dst, ntc, tok0, wkey, dkey):
                for f2 in range(2):
                    for tc in range(ntc):
                        k = pjc[0] % 2
                        pjc[0] += 1
                        for kt in range(KT):
                            MM(pj[k][:, :], w[:, kt, f2 * 128:(f2 + 1) * 128],
                               hT[:, kt, tok0 + tc * 512: tok0 + (tc + 1) * 512],
                               kt == 0, kt == KT - 1, [wkey], [f"pj{k}"])
                        CP("act", dst[:, f2, tc * 512:(tc + 1) * 512], pj[k][:, :], [f"pj{k}"], [dkey])

            for g in range(4):
                DMA("sp", biasS[:], biasT_d[:, 4 * g:4 * g + 4, :], "bias", [], ["bias"])
                proj_fm(wq, QT, 4, HALO, "wq", "QT")
                proj_fm(wk, KTt, 5, 0, "wk", "KT")
                for tt in range(NTE):
                    k = pjc[0] % 2
                    pjc[0] += 1
                    for kt in range(KT):
                        MM(pj[k][:, 0:256], hT[:, kt, tt * 128:(tt + 1) * 128], wv[:, kt, :],
                           kt == 0, kt == KT - 1, ["wv"], [f"pj{k}"])
                    CP("dve", Vx[:, tt, :, 0:64], pj[k][:, 0:256].rearrange("p (h d) -> p h d", h=4),
                       [f"pj{k}"], ["Vx"])
                if g + 1 < 4:
                    LOADQKV(g + 1)
                emit_conv(13 if g == 0 else 9)
                items = [(h4, qt) for h4 in range(4) for qt in range(NTT)]
                n = len(items)

                def S_(i):
                    h4, qt = items[i]
                    s = i % 3
                    p0 = (h4 % 2) * 64
                    f2 = h4 // 2
                    for r in range(5):
                        o = sA3[s][:, r * 128:(r + 1) * 128] if r < 4 else sB3[s][:, 0:128]
                        MM(o, KTt[p0:p0 + 64, f2, (qt + r) * 128:(qt + r + 1) * 128],
                           QT[p0:p0 + 64, f2, qt * 128:(qt + 1) * 128], True, True,
                           ["KT", "QT"], [kA3[s] if r < 4 else kB3[s]])

                def B_(i):
                    h4, qt = items[i]
                    s = i % 3
                    STT(sb[s][:, 0:512], sA3[s][:, :], 0.125, biasS[:, h4, 0:512], ALU.mult, ALU.add,
                        [kA3[s], "bias"], [f"sb{s}a"])
                    STT(sb[s][:, 512:640], sB3[s][:, 0:128], 0.125, biasS[:, h4, 512:640], ALU.mult, ALU.add,
                        [kB3[s], "bias"], [f"sb{s}b"])

                def E_(i):
                    s = i % 3
                    ACTF(PT[s][:, :], sb[s][:, :], AF.Exp, [f"sb{s}a", f"sb{s}b"], [f"PT{s}"])

                def O_(i):
                    h4, qt = items[i]
                    s = i % 2
                    s3 = i % 3
                    for r in range(5):
                        MM(oP[s][:, 0:65], PT[s3][:, r * 128:(r + 1) * 128], Vx[:, qt + r, h4, 0:65],
                           r == 0, r == 4, [f"PT{s3}", "Vx", "Vx1"], [f"oP{s}"])

                def N_(i):
                    h4, qt = items[i]
                    s = i % 2
                    h = g * 4 + h4
                    RCP(rden[s][:, :], oP[s][:, 64:65], [f"oP{s}"], [f"rden{s}"])
                    ACTF(aout[:, qt, h * 64:(h + 1) * 64], oP[s][:, 0:64], AF.Copy, [f"oP{s}", f"rden{s}"],
                         ["aout"], scale=rden[s][:, 0:1])

                S_(0)
                S_(1)
                S_(2)
                B_(0)
                E_(0)
                B_(1)
                E_(1)
                for i in range(n):
                    if i + 2 < n:
                        B_(i + 2)
                    O_(i)
                    if i + 2 < n:
                        E_(i + 2)
                    if i + 3 < n:
                        S_(i + 3)
                    N_(i)
            P.barrier()
            P.flush()

        zc = at("zc", [128, 8, T + 2], BF, 112 * K)
        set_cur(146 * K)
        wC = loc("wC", [128, KT, 256], BF)
        wU = loc("wU", [128, KT, 256], BF)
        wB = loc("wB", [128, KT, 256], BF)
        yb = [loc(f"yb{i}", [128, 512], F32) for i in range(2)]
        cc = [loc(f"cc{i}", [128, 512], F32) for i in range(2)]
        csq = [loc(f"csq{i}", [128, 512], BF) for i in range(2)]
        zt = loc("zt", [128, D], BF)
        junk3 = loc("junk3", [128, DA], BF)
        with ExitStack() as ph:
            pc = [ph.enter_context(nc.psum_tensor(f"pc{i}", [128, 512], F32)) for i in range(4)]
            css = ph.enter_context(nc.psum_tensor("css", [128, 512], F32))
            pcc = [0]
            first_css = [True]

            def fm_chunk(w, f2, lo, nn, wkey):
                k = pcc[0] % 4
                pcc[0] += 1
                for kt in range(KT):
                    MM(pc[k][:, 0:nn], w[:, kt, f2 * 128:(f2 + 1) * 128], hT[:, kt, lo:lo + nn],
                       kt == 0, kt == KT - 1, [wkey], [f"pc{k}"])
                return k

            def LOADW3(w, c0, j, key):
                DMA("pool", w[:], wblk(c0 + j * 256), key, [], [key])

            P.op("dve", lambda e: e.memset(zt[:], 0.0), [], ["zt"])
            for r0 in range(0, NSLOT, 1024):
                DMA("sp", Xs[r0:r0 + 1024, :].rearrange("(k p) c -> p k c", p=128),
                    zt[:, :].unsqueeze(1).to_broadcast([128, 8, D]), f"xz{(r0 // 1024) % 2}", ["zt"], [])
            DMA("sp", Ys[NSLOT:NSLOT + 128, :], zt[:, :], "yz", ["zt"], [])
            LOADW3(wC, 4096, 0, "wC")
            LOADW3(wU, 5120, 0, "wU")
            LOADW3(wB, 3072, 0, "wB")
            sq_todo = list(range(NTT))
            sc_todo = list(range(NTT))
            an_state = {"chain": False}

            def an_act(n):
                for _ in range(n):
                    if sq_todo:
                        tt_ = sq_todo.pop(0)
                        ACTF(junk3[:, :], aout[:, tt_, :], AF.Square, ["aout"], ["junk3", "ssa"],
                             accum_out=ssa[:, tt_:tt_ + 1])
                if not sq_todo and not an_state["chain"]:
                    an_state["chain"] = True
                    rstd_chain(ssa[:, :], None, tmp16[:, :], rstd_a[:, :], NTT, 1.0 / DA, ["ssa"], "rstd_a")

            def an_dve(n):
                if not an_state["chain"]:
                    return
                for _ in range(n):
                    if sc_todo:
                        tt_ = sc_todo.pop(0)
                        TS(aout[:, tt_, :], aout[:, tt_, :], rstd_a[:, tt_:tt_ + 1], 0.0, ALU.mult, ALU.add,
                           ["aout", "rstd_a"], ["aout"])

            for j in range(4):
                zk = lambda ft, tc: f"zc{ft}_{tc}"
                for f2 in range(2):
                    ft = 2 * j + f2
                    k = fm_chunk(wC, f2, HALO - 2, 2, "wC")
                    CP("act", zc[:, ft, 0:2], pc[k][:, 0:2], [f"pc{k}"], [zk(ft, -1)])
                    for tc in range(4):
                        k = fm_chunk(wC, f2, HALO + tc * 512, 512, "wC")
                        CP("act", zc[:, ft, 2 + tc * 512:2 + (tc + 1) * 512], pc[k][:, :], [f"pc{k}"], [zk(ft, tc)])
                        an_act(2)
                if j + 1 < 4:
                    LOADW3(wC, 4096, j + 1, "wC")
                for f2 in range(2):
                    ft = 2 * j + f2
                    k = fm_chunk(wU, f2, HALO - 2, 2, "wU")
                    TT(zc[:, ft, 0:2], pc[k][:, 0:2], zc[:, ft, 0:2], ALU.mult, [f"pc{k}", zk(ft, -1)], [zk(ft, -1)])
                    for tc in range(4):
                        k = fm_chunk(wU, f2, HALO + tc * 512, 512, "wU")
                        sl_ = zc[:, ft, 2 + tc * 512:2 + (tc + 1) * 512]
                        TT(sl_, pc[k][:, :], sl_, ALU.mult, [f"pc{k}", zk(ft, tc)], [zk(ft, tc)])
                        an_dve(2)
                if j + 1 < 4:
                    LOADW3(wU, 5120, j + 1, "wU")
                for f2 in range(2):
                    ft = 2 * j + f2
                    for tc in (3, 2, 1, 0):
                        k = fm_chunk(wB, f2, HALO + tc * 512, 512, "wB")
                        s = tc % 2
                        b0 = tc * 512
                        TS(yb[s][:, :], zc[:, ft, b0 + 2:b0 + 514], cwT[:, ft, 2:3], 0.0, ALU.mult, ALU.add,
                           [zk(ft, tc)], [f"yb{s}"])
                        STT(yb[s][:, :], zc[:, ft, b0 + 1:b0 + 513], cwT[:, ft, 1:2], yb[s][:, :], ALU.mult, ALU.add,
                            [zk(ft, tc), zk(ft, tc - 1), f"yb{s}"], [f"yb{s}"])
                        STT(yb[s][:, :], zc[:, ft, b0:b0 + 512], cwT[:, ft, 0:1], yb[s][:, :], ALU.mult, ALU.add,
                            [zk(ft, tc), zk(ft, tc - 1), f"yb{s}"], [f"yb{s}"])
                        TT(cc[s][:, :], pc[k][:, :], yb[s][:, :], ALU.mult, [f"pc{k}", f"yb{s}"], [f"cc{s}"])
                        ACTF(csq[s][:, :], cc[s][:, :], AF.Square, [f"cc{s}"], [f"csq{s}"])
                        ACTF(zc[:, ft, b0 + 2:b0 + 514], cc[s][:, :], AF.Copy, [f"cc{s}"], [zk(ft, tc)],
                             scale=gcT[:, ft:ft + 1])
                        for i in range(4):
                            tt = tc * 4 + i
                            MM(css[:, tt:tt + 1], csq[s][:, i * 128:(i + 1) * 128], ones[:, 0:1],
                               first_css[0], False, [f"csq{s}"], ["css"], skip=True)
                            first_css[0] = False
                if j + 1 < 4:
                    LOADW3(wB, 3072, j + 1, "wB")
                emit_conv(3)
            an_act(NTT)
            an_dve(NTT)
            assert not sq_todo and not sc_todo
            rstd_chain(css[:, 0:NTT], tmp16[:, :], tmp16b[:, :], rstd_c[:, :], NTT, 1.0 / DC, ["css"], "rstd_c")
            P.barrier()
            P.flush()

        aT = at("aT", [128, 8, T], BF, 0)

        Wo = at("Wo", [128, 16, D], BF, 32 * K)
        set_cur(96 * K)
        xin = [loc(f"xin5{i}", [128, D], F32) for i in range(2)]
        set_cur(146 * K)
        x2t = [loc(f"x2t{i}", [128, D], F32) for i in range(2)]
        h2 = [loc(f"h2{i}", [128, D], BF) for i in range(3)]
        g2b = loc("g2b", [128, D], F32)
        h2T = [loc(f"h2T{i}", [128, KT, 128], BF) for i in range(2)]
        junk = loc("junk5", [128, D], BF)
        wrS = loc("wrS", [128, KT, 36], BF)
        with ExitStack() as ph:
            pa = [ph.enter_context(nc.psum_tensor(f"pa{i}", [128, 512], F32)) for i in range(2)]
            pcv = [ph.enter_context(nc.psum_tensor(f"pcv{i}", [128, 512], F32)) for i in range(2)]
            tp2 = [ph.enter_context(nc.psum_tensor(f"tp2{i}", [128, 8, 128], BF)) for i in range(2)]
            pr = ph.enter_context(nc.psum_tensor("pr", [128, 512], F32))
            pr2 = ph.enter_context(nc.psum_tensor("pr2", [128, 512], F32))
            for i in range(3):
                DMA("sp", Wo[:, 4 * i:4 * i + 4, :], wob_r[:, 4 * i:4 * i + 4, :], f"wo{i}", [], [f"wo{i}"])
            DMA("pool", wrS[:], wr_d.rearrange("p (k c) -> p k c", k=KT), "wr", [], ["wr"])
            DMA("sp", g2b[:], g2b_d, "g2b", [], ["g2b"])
            for tt in range(NTT):
                s = tt % 2
                for ft in range(8):
                    TR(tp2[s][:, ft, :], aout[:, tt, ft * 128:(ft + 1) * 128], ["aout"], [f"tp2{s}"])
                TT(aT[:, :, tt * 128:(tt + 1) * 128], tp2[s][:], gaT[:, 0:8].unsqueeze(2).to_broadcast([128, 8, 128]),
                   ALU.mult, [f"tp2{s}"], ["aT"])
            DMA("sp", Wo[:, 12:16, :], wob_r[:, 12:16, :], "wo3", [], ["wo3", "aout"])
            opc = [0]

            def LOADXIN(tt):
                DMA("sp", xin[tt % 2][:], x_ext[HALO + tt * 128:HALO + (tt + 1) * 128, :], f"xin{tt % 2}", [],
                    [f"xin{tt % 2}"] + (["aout"] if tt < 2 else []))

            LOADXIN(0)

            def OUTPROJ(tt, hooks=None):
                s = tt % 2
                hooks = hooks or {}
                if tt + 1 < NTT:
                    LOADXIN(tt + 1)
                for n4 in range(4):
                    if n4 > 0 and (n4 - 1) in hooks:
                        hooks[n4 - 1]()
                    k = opc[0] % 2
                    opc[0] += 1
                    cs = slice(n4 * 512, (n4 + 1) * 512)
                    for ft in range(8):
                        MM(pa[k][:, :], aT[:, ft, tt * 128:(tt + 1) * 128], Wo[:, ft, cs], ft == 0, ft == 7,
                           [f"wo{ft // 4}", "aT"], [f"pa{k}"])
                    for ft in range(8):
                        MM(pcv[k][:, :], zc[:, ft, 2 + tt * 128:2 + (tt + 1) * 128], Wo[:, 8 + ft, cs],
                           ft == 0, ft == 7, [f"wo{2 + ft // 4}"], [f"pcv{k}"])
                    STT(x2t[s][:, cs], pcv[k][:, :], rstd_c[:, tt:tt + 1], xin[s][:, cs], ALU.mult, ALU.add,
                        [f"pcv{k}", f"xin{s}"], [f"x2t{s}_{n4}"])
                    TT(x2t[s][:, cs], x2t[s][:, cs], pa[k][:, :], ALU.add, [f"pa{k}", f"x2t{s}_{n4}"],
                       [f"x2t{s}_{n4}"])
                x2k = [f"x2t{s}_{n4}" for n4 in range(4)]
                DMA("sp", x2s[tt * 128:(tt + 1) * 128, :], x2t[s][:], f"x2o{s}", x2k, [])
                ACTF(junk[:], x2t[s][:], AF.Square, x2k, ["junk", f"ss{s}"], accum_out=st[s][:, 0:1])
                rstd_chain(st[s][:, 0:1], st[s][:, 1:2], st[s][:, 2:3], st[s][:, 3:4], 1, 1.0 / D,
                           [f"ss{s}"], f"rs{s}")
                STT(h2[tt % 3][:], x2t[s][:], st[s][:, 3:4], g2b[:], ALU.mult, ALU.mult, x2k + [f"rs{s}", "g2b"],
                    [f"h2_{tt % 3}"])

            def TRA(tt):
                s = tt % 2
                s3 = tt % 3
                for h in range(2):
                    for j in range(8):
                        kt = h * 8 + j
                        TR(tp2[h][:, j, :], h2[s3][:, kt * 128:(kt + 1) * 128], [f"h2_{s3}"], [f"tp2{h}"])
                    CP("act", h2T[s][:, h * 8:(h + 1) * 8, :], tp2[h][:], [f"tp2{h}"], [f"h2T{s}{h}"])

            def RA(tt):
                s = tt % 2
                for kt in range(KT):
                    MM(pr[:, 0:36], h2T[s][:, kt, :], wrS[:, kt, :], kt == 0, kt == KT - 1,
                       [f"h2T{s}0", f"h2T{s}1", "wr"], ["pr"])
                TT(lg[:, :], pr[:, 0:36], brb[:, :], ALU.add, ["pr"], ["lg"])
                RED(gmx[:, 0:1], lg[:, 0:4], ALU.max, ["lg"], ["gmax"])
                TT(ohg[:, :], lg[:, 0:4], gmx[:, 0:1].to_broadcast([128, 4]), ALU.is_equal, ["lg", "gmax"], ["ohg"])
                TS(gmx[:, 1:2], gmx[:, 0:1], -1.0, 0.0, ALU.mult, ALU.add, ["gmax"], ["ngmax"])
                ACTF(eg[:, :], lg[:, 0:4], AF.Exp, ["lg", "ngmax"], ["eg", "gsum"], bias=gmx[:, 1:2],
                     accum_out=gmx[:, 2:3])
                TS(pen[:, :], ohg[:, :], 30000.0, -30000.0, ALU.mult, ALU.add, ["ohg"], ["pen"])
                TT(elm[:, :].rearrange("p (g e) -> p g e", g=4), lg[:, 4:36].rearrange("p (g e) -> p g e", g=4),
                   pen[:, 0:4].unsqueeze(2).to_broadcast([128, 4, 8]), ALU.add, ["lg", "pen"], ["elm"])
                P.op("dve", lambda e: e.max(out=m8[:, :], in_=elm[:, :]), ["elm"], ["m8"])
                TT(oh[:, :, :], elm[:, :].unsqueeze(1).to_broadcast([128, 2, NE]),
                   m8[:, 0:2].unsqueeze(2).to_broadcast([128, 2, NE]), ALU.is_equal, ["elm", "m8"], ["oh"])
                TT(Mb[:, :], oh[:, 0, :], oh[:, 1, :], ALU.add, ["oh"], ["Mb"])
                TT(gsc[:, 0:1], m8[:, 1:2], m8[:, 0:1], ALU.subtract, ["m8"], ["gd"])
                ACTF(gsc[:, 1:2], gsc[:, 0:1], AF.Exp, ["gd"], ["ged"])
                TS(gsc[:, 2:3], gsc[:, 1:2], 1.0, 0.0, ALU.add, ALU.add, ["ged"], ["gden"])
                TT(gsc[:, 3:4], gsc[:, 2:3], gmx[:, 2:3], ALU.mult, ["gden", "gsum"], ["gdg"])
                RCP(gw[:, tt, 0:1], gsc[:, 3:4], ["gdg"], [f"gw{tt}a"])
                TT(gw[:, tt, 1:2], gw[:, tt, 0:1], gsc[:, 1:2], ALU.mult, [f"gw{tt}a", "ged"], [f"gw{tt}b"])

            def RB(tt):
                s3 = tt % 3
                MM(pr2[:, 64:96], tri[:, :], Mb[:, :], True, True, ["Mb"], ["pr2"])
                MM(pr2[:, 128:160], ones[:, :], Mb[:, :], True, True, ["Mb"], ["pr2"])
                TT(rk[:, :], pr2[:, 64:96], cnt[:, :], ALU.add, ["pr2", "cnt"], ["rk"])
                TT(cnt[:, :], cnt[:, :], pr2[:, 128:160], ALU.add, ["pr2", "cnt"], ["cnt"])
                TT(tmpe[:, :], rk[:, :], ecap[:, :], ALU.add, ["rk"], ["tmpe"])
                TT(prod[:, :, :], oh[:, :, :], tmpe[:, :].unsqueeze(1).to_broadcast([128, 2, NE]), ALU.mult,
                   ["oh", "tmpe"], ["prod"])
                RED(slf[:, :], prod[:, :, :], ALU.add, ["prod"], ["slf"])
                TT(prod[:, :, :], oh[:, :, :], rk[:, :].unsqueeze(1).to_broadcast([128, 2, NE]), ALU.mult,
                   ["oh", "rk", "slf"], ["prod"])
                RED(rr[:, :], prod[:, :, :], ALU.add, ["prod"], ["rr"])
                TS(tov[:, :], rr[:, :], -float(CAP - 1), 0.0, ALU.add, ALU.max, ["rr"], ["tov"])
                TS(tov[:, :], tov[:, :], 1.0, 1.0, ALU.min, ALU.mult, ["tov"], ["tov"])
                TS(dsl[:, :], slf[:, :], -1.0, float(NSLOT), ALU.mult, ALU.add, ["slf"], ["dsl"])
                TT(dsl[:, :], dsl[:, :], tov[:, :], ALU.mult, ["dsl", "tov"], ["dsl"])
                TT(slf[:, :], slf[:, :], dsl[:, :], ALU.add, ["slf", "dsl"], ["slf"])
                CP("dve", sl_i[:, tt, :], slf[:, :], ["slf"], [f"sl{tt}"])
                if 1 <= tt <= 9:
                    emit_conv(1)
                for kk in range(2):
                    P.dma("pool", lambda e, kk=kk, s3=s3, tt=tt: e.indirect_dma_start(
                        out=Xs[:, :], out_offset=bass.IndirectOffsetOnAxis(ap=sl_i[:, tt, kk:kk + 1], axis=0),
                        in_=h2[s3][:, :], in_offset=None, bounds_check=NSLOT - 1, oob_is_err=False),
                        f"sc{s3}{kk}", [f"h2_{s3}", f"sl{tt}"], [])

            OUTPROJ(0)
            OUTPROJ(1, hooks={1: (lambda: TRA(0)), 2: (lambda: RA(0))})
            for tt in range(NTT):
                hk = {0: (lambda t=tt: RB(t))}
                if tt + 1 < NTT:
                    hk[1] = (lambda t=tt + 1: TRA(t))
                    hk[2] = (lambda t=tt + 1: RA(t))
                if tt + 2 < NTT:
                    OUTPROJ(tt + 2, hooks=hk)
                else:
                    for kk_ in (0, 1, 2):
                        if kk_ in hk:
                            hk[kk_]()
            set_cur(0)
            wg = [loc(f"wg{i}", [128, KT, DE], BF) for i in range(2)]
            wu = [loc(f"wu{i}", [128, KT, DE], BF) for i in range(2)]
            wd = [loc(f"wd{i}", [128, 4, D], BF) for i in range(2)]
            pk_ = ["aT", "wo0", "wo1", "wo2", "wo3"]
            DMA("sp", wg[0][:], wgb[0].rearrange("p (k c) -> p k c", k=KT), "wg0", [], ["wg0"] + pk_)
            DMA("sp", wu[0][:], wub[0].rearrange("p (k c) -> p k c", k=KT), "wu0", [], ["wu0"] + pk_)
            DMA("sp", wd[0][:], wdb[0].rearrange("p (j c) -> p j c", j=4), "wd0", [], ["wd0"] + pk_)
            if dbg is not None:
                P.op("dve", lambda e: e.tensor_copy(out=x2t[0][:, 0:NTT * 8].rearrange("p (t k) -> p t k", k=8)[:, :, 0:2],
                                                    in_=sl_i[:, :, :]), [], ["dbgt"] + [f"x2t0_{n4}" for n4 in range(4)])
                P.op("dve", lambda e: e.tensor_copy(out=x2t[0][:, 0:NTT * 8].rearrange("p (t k) -> p t k", k=8)[:, :, 2:4],
                                                    in_=gw[:, :, :]), ["dbgt"], ["dbgt"])
                DMA("sp", dbg, x2t[0][:, 0:NTT * 8].rearrange("p (t k) -> p t k", k=8), "dbg", ["dbgt"], [])
            P.barrier()
            P.flush()

        if stage >= 6:
            set_cur(96 * K)
            xr = [loc(f"xr{i}", [128, NSUB, D], BF) for i in range(2)]
            xbT = [loc(f"xbT{i}", [128, KT, CAP], BF) for i in range(2)]
            aTe = [loc(f"aTe{i}", [128, 4, CAP], BF) for i in range(2)]
            sg = [loc(f"sg{i}", [128, CAP], F32) for i in range(2)]
            ysb = [loc(f"ysb{i}", [128, D], BF) for i in range(4)]
            with ExitStack() as ph:
                tpe = [ph.enter_context(nc.psum_tensor(f"tpe{i}", [128, 8, 128], BF)) for i in range(2)]
                pg_ = [ph.enter_context(nc.psum_tensor(f"pg{i}", [128, 512], F32)) for i in range(2)]
                pu_ = [ph.enter_context(nc.psum_tensor(f"pu{i}", [128, 512], F32)) for i in range(2)]
                py = [ph.enter_context(nc.psum_tensor(f"py{i}", [128, 512], F32)) for i in range(2)]
                c1 = [0, 0, 0, 0]

                def LOADW(e_):
                    s = e_ % 2
                    if e_ < NCONV:
                        DMA("sp", wg[s][:], wgb[e_].rearrange("p (k c) -> p k c", k=KT), f"wg{s}", [], [f"wg{s}"])
                        DMA("sp", wu[s][:], wub[e_].rearrange("p (k c) -> p k c", k=KT), f"wu{s}", [], [f"wu{s}"])
                        DMA("sp", wd[s][:], wdb[e_].rearrange("p (j c) -> p j c", j=4), f"wd{s}", [], [f"wd{s}"])
                    else:
                        DMA("pool", wg[s][:], w_gate[e_].rearrange("p (k c) -> p k c", k=KT), f"wgp{s}", [], [f"wg{s}"])
                        DMA("pool", wu[s][:], w_up[e_].rearrange("p (k c) -> p k c", k=KT), f"wup{s}", [], [f"wu{s}"])
                        DMA("pool", wd[s][:], w_down[e_].rearrange("p (j c) -> p j c", j=4), f"wdp{s}", [], [f"wd{s}"])

                def LOADX(e_):
                    s = e_ % 2
                    nf = CAP // 128
                    DMA("sp", xr[s][:, 0:nf, :],
                        Xs[e_ * CAP:e_ * CAP + nf * 128, :].rearrange("(sub p) c -> p sub c", p=128),
                        f"xr{s}", [], [f"xr{s}a"])
                    if CAP % 128:
                        rem = CAP % 128
                        DMA("sp", xr[s][0:rem, nf, :], Xs[e_ * CAP + nf * 128:(e_ + 1) * CAP, :],
                            f"xr{s}b", [], [f"xr{s}b"])

                def TRX(e_):
                    s = e_ % 2
                    for sub in range(NSUB):
                        rs = RSZ[sub]
                        xkey = f"xr{s}a" if rs == 128 else f"xr{s}b"
                        for h in range(2):
                            k = c1[0] % 2
                            c1[0] += 1
                            for j in range(8):
                                kt = h * 8 + j
                                P.op("pe", lambda e, k=k, j=j, s=s, sub=sub, kt=kt, rs=rs: e.transpose(
                                    out=tpe[k][:, j, 0:rs], in_=xr[s][0:rs, sub, kt * 128:(kt + 1) * 128],
                                    identity=ident[0:rs, 0:rs]), [xkey], [f"tpe{k}"])
                            CP("act" if k == 0 else "dve", xbT[s][:, h * 8:(h + 1) * 8, sub * 128:sub * 128 + rs],
                               tpe[k][:, :, 0:rs], [f"tpe{k}"], [f"xbT{s}_{sub}{h}"])

                def GU(e_):
                    s = e_ % 2
                    xk = [f"xbT{s}_{a}{b}" for a in range(NSUB) for b in range(2)]
                    for jt in range(4):
                        k = c1[1] % 2
                        c1[1] += 1
                        for kt in range(KT):
                            MM(pg_[k][:, 0:CAP], wg[s][:, kt, jt * 128:(jt + 1) * 128], xbT[s][:, kt, :],
                               kt == 0, kt == KT - 1, xk + [f"wg{s}"], [f"pg{k}"])
                        for kt in range(KT):
                            MM(pu_[k][:, 0:CAP], wu[s][:, kt, jt * 128:(jt + 1) * 128], xbT[s][:, kt, :],
                               kt == 0, kt == KT - 1, xk + [f"wu{s}"], [f"pu{k}"])
                        ACTF(sg[k][:, :], pg_[k][:, 0:CAP], AF.Silu, [f"pg{k}"], [f"sg{k}"])
                        TT(aTe[s][:, jt, :], sg[k][:, :], pu_[k][:, 0:CAP], ALU.mult, [f"pu{k}", f"sg{k}"],
                           [f"aTe{s}_{jt}"])

                def YY(e_):
                    s = e_ % 2
                    ak = [f"aTe{s}_{jt}" for jt in range(4)]
                    for sub in range(NSUB):
                        rs = RSZ[sub]
                        ys = c1[2] % 4
                        c1[2] += 1
                        for n4 in range(4):
                            k = c1[3] % 2
                            c1[3] += 1
                            for jt in range(4):
                                MM(py[k][0:rs, :], aTe[s][:, jt, sub * 128:sub * 128 + rs],
                                   wd[s][:, jt, n4 * 512:(n4 + 1) * 512], jt == 0, jt == 3,
                                   ak + [f"wd{s}"], [f"py{k}"])
                            CP("act" if n4 % 2 == 0 else "dve", ysb[ys][0:rs, n4 * 512:(n4 + 1) * 512], py[k][0:rs, :],
                               [f"py{k}"], [f"ysb{ys}_{n4}"])
                        r0 = e_ * CAP + sub * 128
                        DMA("pool", Ys[r0:r0 + rs, :], ysb[ys][0:rs, :], f"yo{ys}", [f"ysb{ys}_{n4}" for n4 in range(4)], [])

                LOADX(0)
                TRX(0)
                for e_ in range(NE):
                    if e_ + 1 < NE:
                        LOADW(e_ + 1)
                        LOADX(e_ + 1)
                    GU(e_)
                    if e_ + 1 < NE:
                        TRX(e_ + 1)
                    YY(e_)
                    if e_ == NE - 4:
                        set_cur(165 * K)
                        x2t = [loc(f"x2f{i}", [128, D], F32) for i in range(3)]
                        gfb = loc("gfb", [128, D], F32)
                        DMA("sp", gfb[:], gfb_d, "gfb", [], ["gfb"])
                        for t7 in range(3):
                            DMA("sp", x2t[t7][:], x2s[t7 * 128:(t7 + 1) * 128, :], f"x2f{t7}", [], [f"x2f{t7}"])
                P.barrier()
                P.flush()

            set_cur(0)
            ya = [loc(f"ya{i}", [128, D], BF) for i in range(3)]
            ybb = [loc(f"ybb{i}", [128, D], BF) for i in range(3)]
            x3 = [loc(f"x3{i}", [128, D], F32) for i in range(2)]
            ot = [loc(f"ot{i}", [128, D], F32) for i in range(2)]
            junk = loc("junk7", [128, D], BF)

            def LOAD7(tt):
                s3 = tt % 3
                if tt >= 3:
                    DMA("sp", x2t[s3][:], x2s[tt * 128:(tt + 1) * 128, :], f"x2f{s3}", [], [f"x2f{s3}"])
                for kk, dst in ((0, ya), (1, ybb)):
                    P.dma("pool", lambda e, kk=kk, dst=dst, s3=s3, tt=tt: e.indirect_dma_start(
                        out=dst[s3][:, :], out_offset=None, in_=Ys[:, :],
                        in_offset=bass.IndirectOffsetOnAxis(ap=sl_i[:, tt, kk:kk + 1], axis=0)),
                        f"yg{s3}{kk}", [], [f"yg{s3}{kk}"])

            def X37(tt):
                s = tt % 2
                s3 = tt % 3
                STT(x3[s][:], ya[s3][:], gw[:, tt, 0:1], x2t[s3][:], ALU.mult, ALU.add, [f"yg{s3}0", f"x2f{s3}"],
                    [f"x3{s}"])
                STT(x3[s][:], ybb[s3][:], gw[:, tt, 1:2], x3[s][:], ALU.mult, ALU.add, [f"yg{s3}1", f"x3{s}"], [f"x3{s}"])
                ACTF(junk[:], x3[s][:], AF.Square, [f"x3{s}"], ["junk", f"ss{s}"], accum_out=st[s][:, 0:1])
                ACTF(st[s][:, 2:3], st[s][:, 0:1], AF.Sqrt, [f"ss{s}"], [f"rs{s}_s"], scale=1.0 / D, bias=epsT[:, 0:1])

            def FIN7(tt):
                s = tt % 2
                RCP(st[s][:, 3:4], st[s][:, 2:3], [f"rs{s}_s"], [f"rs{s}"])
                STT(ot[s][:], x3[s][:], st[s][:, 3:4], gfb[:], ALU.mult, ALU.mult, [f"x3{s}", f"rs{s}", "gfb"], [f"ot{s}"])
                DMA("sp", out_d[tt * 128:(tt + 1) * 128, :], ot[s][:], f"oo{s}", [f"ot{s}"], [])

            LOAD7(0)
            LOAD7(1)
            X37(0)
            for tt in range(NTT):
                if tt + 2 < NTT:
                    LOAD7(tt + 2)
                if tt + 1 < NTT:
                    X37(tt + 1)
                FIN7(tt)
            P.barrier()
            P.flush()
    return nc


def _bias_table(rel_bias):
    kk = np.arange(128)[:, None, None]
    r = np.arange(5)[None, :, None]
    qq = np.arange(128)[None, None, :]
    dist = 512 + qq - r * 128 - kk
    idx = np.clip(dist, -256, 256) + 256
    dch = (8 + qq // 64) - (2 * r + kk // 64)
    ok = (dch >= 0) & (dch <= 8)
    tab = rel_bias[:, idx]
    tab = np.where(ok[None], tab, np.float32(-1e30)).astype(np.float32)
    return np.ascontiguousarray(tab.transpose(1, 0, 2, 3).reshape(128, 16, 640))


def kernel(x, norm1, w_in, rel_bias, conv_w, g_out_attn, g_out_conv, w_out, norm2,
           w_router_group, b_router_group, w_router_expert, b_router_expert,
           w_gate, w_up, w_down, norm_final):
    stage = int(os.environ.get("MK_STAGE", "99"))
    f = lambda a: np.ascontiguousarray(np.asarray(a, dtype=np.float32))
    x = f(x)
    B, S, _ = x.shape
    shared = dict(
        w_in=f(np.asarray(w_in[0]).reshape(KT, 128, 24, 256).transpose(2, 1, 0, 3)).reshape(24, 128, KT * 256),
        g1T=f(np.asarray(norm1[0]).reshape(KT, 128).T),
        biasT=_bias_table(f(rel_bias[0])),
        cwT=f(np.asarray(conv_w[0]).reshape(3, 8, 128).transpose(2, 1, 0)),
        gaT=f(np.asarray(g_out_attn[0]).reshape(8, 128).T),
        gcT=f(np.asarray(g_out_conv[0]).reshape(8, 128).T),
        w_out=f(np.asarray(w_out[0]).reshape(KT, 128, D).transpose(1, 0, 2)).reshape(128, KT * D),
        g2b=f(np.broadcast_to(np.asarray(norm2[0])[None, :], (128, D))),
        gfb=f(np.broadcast_to(np.asarray(norm_final)[None, :], (128, D))),
        wr=f(np.concatenate([np.asarray(w_router_group[0]), np.asarray(w_router_expert[0])], axis=1)
             .reshape(KT, 128, 36).transpose(1, 0, 2)).reshape(128, KT * 36),
        brb=f(np.broadcast_to(np.concatenate([np.asarray(b_router_group[0]),
                                              np.asarray(b_router_expert[0])])[None, :], (128, 36))),
        w_gate=f(np.asarray(w_gate[0]).reshape(NE, KT, 128, DE).transpose(0, 2, 1, 3)).reshape(NE, 128, KT * DE),
        w_up=f(np.asarray(w_up[0]).reshape(NE, KT, 128, DE).transpose(0, 2, 1, 3)).reshape(NE, 128, KT * DE),
        w_down=f(np.asarray(w_down[0]).reshape(NE, 4, 128, D).transpose(0, 2, 1, 3)).reshape(NE, 128, 4 * D),
        ident=np.eye(128, dtype=np.float32).astype(bf16),
        tri=np.triu(np.ones((128, 128), np.float32), 1).astype(bf16),
        ones=np.ones((128, 128), np.float32).astype(bf16),
        ecap=f(np.broadcast_to((np.arange(NE) * CAP)[None, :], (128, NE))),
    )
    in_maps = []
    for c in range(NCORES):
        b, half = c // 2, c % 2
        xe = np.zeros((TE, D), np.float32)
        if half == 1:
            xe[:HALO] = x[b, T - HALO:T]
        xe[HALO:] = x[b, half * T:(half + 1) * T]
        val = np.ones((NTE, 128), np.float32)
        if half == 0:
            val[:HALO // 128] = 0.0
        m = dict(shared)
        m["x_ext"] = xe
        m["valid"] = np.ascontiguousarray(val.T)
        in_maps.append(m)
    nc = build(stage)
    res = run_bass_kernel_spmd(nc, in_maps, core_ids=list(range(NCORES)))
    key = "out" if stage >= 99 else "x2s"
    out = np.empty((B, S, D), np.float32)
    for c in range(NCORES):
        b, half = c // 2, c % 2
        out[b, half * T:(half + 1) * T] = res.results[c][key]
    if stage < 99:
        kernel.dbg = [res.results[c]["dbg"] for c in range(NCORES)]
    return out
```

```python
import os
from contextlib import ExitStack

import ml_dtypes
import numpy as np

import concourse.bass as bass
import concourse.mybir as mybir
from concourse.bass_utils import run_bass_kernel_spmd

F32 = mybir.dt.float32
BF = mybir.dt.bfloat16
I32 = mybir.dt.int32
ALU = mybir.AluOpType
AF = mybir.ActivationFunctionType
AX = mybir.AxisListType
bf16 = ml_dtypes.bfloat16

NCORES = 8
D = 2048
KT = 16
T = 2048
HALO = 512
TE = T + HALO
NTT = T // 128
NTE = TE // 128
DA = 1024
DC = 1024
NE = 32
DE = 512
CAP = 320
NSUB = (CAP + 127) // 128
RSZ = [min(128, CAP - 128 * i) for i in range(NSUB)]
NSLOT = NE * CAP
EPS = 1e-6
BIGT = 20000.0
VW = 66

ENGS = ("pe", "dve", "act", "pool", "sp")


class Op:
    __slots__ = ("eng", "emit", "deps", "is_dma", "sem_key", "val", "signal")

    def __init__(self, eng, emit, is_dma, sem_key=None, val=0):
        self.eng = eng
        self.emit = emit
        self.deps = []
        self.is_dma = is_dma
        self.sem_key = sem_key
        self.val = val
        self.signal = False


class Prog:
    def __init__(self, nc, stack):
        self.nc = nc
        self.pending = {e: [] for e in ENGS}
        self.cnt = {e: 0 for e in ENGS}
        self.dma_cnt = {}
        self.sems = {}
        self.waited = {e: {} for e in ENGS}
        self.last_w = {}
        self.readers = {}
        self.last_op = {e: None for e in ENGS}
        self._stack = stack
        self._old_dmas = []

    def _sem(self, name):
        if name not in self.sems:
            self.sems[name] = self._stack.enter_context(self.nc.semaphore(name))
        return self.sems[name]

    def _track(self, op, reads, writes, extra):
        deps = []
        for k in reads:
            w = self.last_w.get(k)
            if w is not None:
                deps.append(w)
        for k in writes:
            w = self.last_w.get(k)
            if w is not None:
                deps.append(w)
            deps.extend(self.readers.get(k, ()))
        deps.extend(extra)
        seen = set()
        for d in deps:
            if d is None or d is op or id(d) in seen:
                continue
            if d.eng == "pe" and op.eng == "pe" and not d.is_dma and not op.is_dma:
                continue
            seen.add(id(d))
            op.deps.append(d)
            if not d.is_dma:
                d.signal = True
        for k in reads:
            self.readers.setdefault(k, []).append(op)
        for k in writes:
            self.last_w[k] = op
            self.readers[k] = []

    def op(self, eng, emit, reads=(), writes=(), deps=()):
        o = Op(eng, emit, False)
        self._track(o, reads, writes, deps)
        self.pending[eng].append(o)
        self.last_op[eng] = o
        return o

    def dma(self, eng, emit, sem_key, reads=(), writes=(), deps=()):
        c = self.dma_cnt.get(sem_key, 0) + 16
        self.dma_cnt[sem_key] = c
        o = Op(eng, emit, True, sem_key="d_" + sem_key, val=c)
        self._track(o, reads, writes, deps)
        self.pending[eng].append(o)
        return o

    def barrier(self):
        lasts = [self.last_op[e] for e in ENGS if self.last_op[e] is not None]
        best = {}
        for o in [o for e in ENGS for o in self.pending[e] if o.is_dma] + self._old_dmas:
            if o.sem_key not in best or best[o.sem_key].val < o.val:
                best[o.sem_key] = o
        alld = lasts + list(best.values())
        for e in ENGS:
            o = Op(e, None, False)
            for d in alld:
                o.deps.append(d)
                if not d.is_dma:
                    d.signal = True
            self.pending[e].append(o)
        self.last_w = {}
        self.readers = {}

    def flush(self):
        nc = self.nc
        for e in ENGS:
            for o in self.pending[e]:
                if not o.is_dma and o.signal:
                    self.cnt[e] += 1
                    o.val = self.cnt[e]
                    o.sem_key = "e_" + e
        for e in ENGS:
            for o in self.pending[e]:
                if o.sem_key is not None:
                    self._sem(o.sem_key)
        pend = self.pending
        self.pending = {e: [] for e in ENGS}
        best = {}
        for o in [o for e in ENGS for o in pend[e] if o.is_dma] + self._old_dmas:
            if o.sem_key not in best or best[o.sem_key].val < o.val:
                best[o.sem_key] = o
        self._old_dmas = list(best.values())

        def run(e, engine):
            waited = self.waited[e]
            for o in pend[e]:
                for d in sorted(o.deps, key=lambda d_: -d_.val):
                    if waited.get(d.sem_key, 0) >= d.val:
                        continue
                    engine.wait_ge(self.sems[d.sem_key], d.val)
                    waited[d.sem_key] = d.val
                if o.emit is None:
                    continue
                inst = o.emit(engine)
                if o.is_dma:
                    inst.then_inc(self.sems[o.sem_key], 16)
                elif o.signal:
                    inst.then_inc(self.sems[o.sem_key], 1)

        with nc.Block() as block:
            if pend["pe"]:
                @block.tensor
                def _(eng):
                    run("pe", eng)
            if pend["dve"]:
                @block.vector
                def _(eng):
                    run("dve", eng)
            if pend["act"]:
                @block.scalar
                def _(eng):
                    run("act", eng)
            if pend["pool"]:
                @block.gpsimd
                def _(eng):
                    run("pool", eng)
            if pend["sp"]:
                @block.sync
                def _(eng):
                    run("sp", eng)


def build(stage=99):
    nc = bass.Bass("TRN2", target_bir_lowering=False)

    def din(name, shape, dt=F32):
        return nc.dram_tensor(name, shape, dt, kind="ExternalInput").ap()

    x_ext = din("x_ext", [TE, D])
    valid_d = din("valid", [128, NTE])
    w_in = din("w_in", [24, 128, KT * 256])
    g1T_d = din("g1T", [128, KT])
    biasT_d = din("biasT", [128, 16, 640])
    cwT_d = din("cwT", [128, 8, 3])
    gaT_d = din("gaT", [128, 8])
    gcT_d = din("gcT", [128, 8])
    w_out = din("w_out", [128, KT * D])
    g2b_d = din("g2b", [128, D])
    gfb_d = din("gfb", [128, D])
    wr_d = din("wr", [128, KT * 36])
    brb_d = din("brb", [128, 36])
    w_gate = din("w_gate", [NE, 128, KT * DE])
    w_up = din("w_up", [NE, 128, KT * DE])
    w_down = din("w_down", [NE, 128, 4 * D])
    ident_d = din("ident", [128, 128], BF)
    tri_d = din("tri", [128, 128], BF)
    ones_d = din("ones", [128, 128], BF)
    ecap_d = din("ecap", [128, NE])
    out_d = nc.dram_tensor("out", [T, D], F32, kind="ExternalOutput").ap()
    if stage < 99:
        x2s = nc.dram_tensor("x2s", [T, D], F32, kind="ExternalOutput").ap()
        dbg = nc.dram_tensor("dbg", [128, NTT, 8], F32, kind="ExternalOutput").ap()
    else:
        x2s = nc.dram_tensor("x2s", [T, D], F32).ap()
        dbg = None
    NCONV = 19
    wgb = nc.dram_tensor("wgb", [NCONV, 128, KT * DE], BF).ap()
    wub = nc.dram_tensor("wub", [NCONV, 128, KT * DE], BF).ap()
    wdb = nc.dram_tensor("wdb", [NCONV, 128, 4 * D], BF).ap()
    wob = nc.dram_tensor("wob", [128, KT * D], BF).ap()
    Xs = nc.dram_tensor("Xs", [NSLOT, D], BF).ap()
    Ys = nc.dram_tensor("Ys", [NSLOT + 128, D], BF).ap()

    def wblk(c0):
        return w_in[c0 // 256].rearrange("p (k c) -> p k c", k=KT)

    w_out_r = w_out.rearrange("p (f c) -> p f c", f=KT)
    wob_r = wob.rearrange("p (f c) -> p f c", f=KT)

    BASE = 16640
    TOP = 229376
    SMALL = TOP - 6144
    esz = {F32: 4, BF: 2, I32: 4}
    cur = [BASE]
    uid = [0]

    def at(name, shape, dt, off):
        uid[0] += 1
        n = 1
        for s in shape[1:]:
            n *= s
        assert BASE + off + n * esz[dt] <= TOP, (name, off, n * esz[dt])
        return nc.alloc_sbuf_tensor_at(f"{name}_{uid[0]}", list(shape), dt, offset=BASE + off)

    def loc(name, shape, dt, limit=SMALL):
        n = 1
        for s in shape[1:]:
            n *= s
        sz = (n * esz[dt] + 31) // 32 * 32
        off = cur[0]
        assert off + sz <= limit, (name, off, sz, limit)
        cur[0] = off + sz
        uid[0] += 1
        return nc.alloc_sbuf_tensor_at(f"{name}_{uid[0]}", list(shape), dt, offset=off)

    def set_cur(off):
        cur[0] = BASE + off

    K = 1024

    with ExitStack() as top:
        P = Prog(nc, top)

        def MM(out, lhsT, rhs, start, stop, reads, writes, skip=False):
            if skip:
                P.op("pe", lambda e: e.matmul(out, lhsT=lhsT, rhs=rhs, start=start, stop=stop,
                                              skip_group_check=True), reads, writes)
            else:
                P.op("pe", lambda e: e.matmul(out, lhsT=lhsT, rhs=rhs, start=start, stop=stop),
                     reads, writes)

        def TR(out, in_, reads, writes):
            P.op("pe", lambda e: e.transpose(out=out, in_=in_, identity=ident[:]), reads, writes)

        def ACTF(out, in_, func, reads, writes, **kw):
            P.op("act", lambda e: e.activation(out=out, in_=in_, func=func, **kw), reads, writes)

        def TS(out, in0, s1, s2, op0, op1, reads, writes, eng="dve"):
            P.op(eng, lambda e: e.tensor_scalar(out=out, in0=in0, scalar1=s1, scalar2=s2, op0=op0, op1=op1),
                 reads, writes)

        def STT(out, in0, scalar, in1, op0, op1, reads, writes, eng="dve"):
            P.op(eng, lambda e: e.scalar_tensor_tensor(out=out, in0=in0, scalar=scalar, in1=in1,
                                                       op0=op0, op1=op1), reads, writes)

        def TT(out, in0, in1, op, reads, writes, eng="dve"):
            P.op(eng, lambda e: e.tensor_tensor(out=out, in0=in0, in1=in1, op=op), reads, writes)

        def CP(eng, out, in_, reads, writes):
            if eng == "act":
                P.op("act", lambda e: e.copy(out=out, in_=in_), reads, writes)
            else:
                P.op(eng, lambda e: e.tensor_copy(out=out, in_=in_), reads, writes)

        def RED(out, in_, op, reads, writes):
            P.op("dve", lambda e: e.tensor_reduce(out=out, in_=in_, axis=AX.X, op=op), reads, writes)

        def RCP(out, in_, reads, writes):
            P.op("dve", lambda e: e.reciprocal(out=out, in_=in_), reads, writes)

        def DMA(q, out, in_, key, reads, writes):
            return P.dma(q, lambda e: e.dma_start(out=out, in_=in_), key, reads, writes)

        def rstd_chain(ss, tmp, srt, out, n, inv_n, rk, wk):
            ACTF(srt, ss, AF.Sqrt, rk, [wk + "_s"], scale=inv_n, bias=epsT[:, 0:1])
            RCP(out, srt, [wk + "_s"], [wk])

        conv_list = []
        for q_ in range(4):
            conv_list.append((wob[:, q_ * 4 * D:(q_ + 1) * 4 * D], w_out[:, q_ * 4 * D:(q_ + 1) * 4 * D]))
        for e_ in range(NCONV):
            conv_list.append((wgb[e_], w_gate[e_]))
            conv_list.append((wub[e_], w_up[e_]))
            conv_list.append((wdb[e_], w_down[e_]))
        conv_pos = [0]

        def emit_conv(n):
            for _ in range(n):
                if conv_pos[0] >= len(conv_list):
                    return
                o_, i_ = conv_list[conv_pos[0]]
                DMA("pool", o_, i_, f"cv{conv_pos[0] % 4}", [], [])
                conv_pos[0] += 1

        cur[0] = SMALL
        LIM = TOP
        ident = loc("ident", [128, 128], BF, LIM)
        tri = loc("tri", [128, 128], BF, LIM)
        ones = loc("ones", [128, 128], BF, LIM)
        g1T = loc("g1T", [128, KT], F32, LIM)
        gaT = loc("gaT", [128, 8], F32, LIM)
        gcT = loc("gcT", [128, 8], F32, LIM)
        cwT = loc("cwT", [128, 8, 3], F32, LIM)
        valid = loc("valid", [128, NTE], F32, LIM)
        ecap = loc("ecap", [128, NE], F32, LIM)
        brb = loc("brb", [128, 36], F32, LIM)
        sl_i = loc("sl_i", [128, NTT, 2], I32, LIM)
        gw = loc("gw", [128, NTT, 2], F32, LIM)
        rstd_c = loc("rstd_c", [128, NTT], F32, LIM)
        rstd_a = loc("rstd_a", [128, NTT], F32, LIM)
        ssa = loc("ssa", [128, NTT], F32, LIM)
        tmp16 = loc("tmp16", [128, NTT], F32, LIM)
        tmp16b = loc("tmp16b", [128, NTT], F32, LIM)
        cnt = loc("cnt", [128, NE], F32, LIM)
        epsT = loc("epsT", [128, 1], F32, LIM)
        st6 = [loc(f"st6{i}", [128, 4], F32, LIM) for i in range(6)]
        st = [loc(f"st{i}", [128, 4], F32, LIM) for i in range(2)]
        rden = [loc(f"rden{i}", [128, 1], F32, LIM) for i in range(2)]
        lg = loc("lg", [128, 36], F32, LIM)
        gmx = loc("gmx", [128, 4], F32, LIM)
        ohg = loc("ohg", [128, 4], F32, LIM)
        pen = loc("pen", [128, 4], F32, LIM)
        eg = loc("eg", [128, 4], F32, LIM)
        elm = loc("elm", [128, NE], F32, LIM)
        m8 = loc("m8", [128, 8], F32, LIM)
        oh = loc("oh", [128, 2, NE], F32, LIM)
        prod = loc("prod", [128, 2, NE], F32, LIM)
        Mb = loc("Mb", [128, NE], BF, LIM)
        rk = loc("rk", [128, NE], F32, LIM)
        tmpe = loc("tmpe", [128, NE], F32, LIM)
        slf = loc("slf", [128, 2], F32, LIM)
        rr = loc("rr", [128, 2], F32, LIM)
        tov = loc("tov", [128, 2], F32, LIM)
        dsl = loc("dsl", [128, 2], F32, LIM)
        gsc = loc("gsc", [128, 4], F32, LIM)

        for (t_, d_, k_) in ((ident, ident_d, "c0"), (tri, tri_d, "c1"), (ones, ones_d, "c2"), (g1T, g1T_d, "c3"),
                             (gaT, gaT_d, "c4"), (gcT, gcT_d, "c5"), (cwT, cwT_d, "c6"), (valid, valid_d, "c7"),
                             (ecap, ecap_d, "c8"), (brb, brb_d, "c9")):
            DMA("sp", t_[:], d_, k_, [], [k_])
        P.op("dve", lambda e: e.memset(cnt[:], 0.0), [], ["cnt"])
        P.op("dve", lambda e: e.memset(epsT[:], EPS), [], ["epsT"])
        P.barrier()

        hT = at("hT", [128, KT, TE], BF, 0)
        set_cur(112 * K)
        wq = loc("wq", [128, KT, 256], BF)
        wk = loc("wk", [128, KT, 256], BF)
        wv = loc("wv", [128, KT, 256], BF)

        def LOADQKV(g):
            DMA("pool", wq[:], wblk(g * 256), "wq", [], ["wq"])
            DMA("pool", wk[:], wblk(1024 + g * 256), "wk", [], ["wk"])
            DMA("pool", wv[:], wblk(2048 + g * 256), "wv", [], ["wv"])

        set_cur(137 * K)
        xin = [loc(f"xin{i}", [128, D], F32) for i in range(6)]
        xs = [loc(f"xs{i}", [128, D], BF) for i in range(3)]
        junk = loc("junk", [128, D], BF)
        with ExitStack() as ph:
            tp = [[ph.enter_context(nc.psum_tensor(f"tp{s}{h}", [128, 8, 128], BF)) for h in range(2)]
                  for s in range(2)]

            def L1(tt):
                s6 = tt % 6
                DMA("sp", xin[s6][:], x_ext[tt * 128:(tt + 1) * 128, :], f"xin{s6}", [], [f"xin{s6}"])

            def A1(tt):
                s6 = tt % 6
                ACTF(junk[:], xin[s6][:], AF.Square, [f"xin{s6}"], ["junk", f"ss{s6}"], accum_out=st6[s6][:, 0:1])

            def B1(tt):
                s6 = tt % 6
                rstd_chain(st6[s6][:, 0:1], None, st6[s6][:, 2:3], st6[s6][:, 3:4], 1, 1.0 / D, [f"ss{s6}"], f"rs{s6}")

            def C1(tt):
                s6 = tt % 6
                s3 = tt % 3
                s = tt % 2
                TS(xs[s3][:], xin[s6][:], st6[s6][:, 3:4], 0.0, ALU.mult, ALU.add, [f"xin{s6}", f"rs{s6}"],
                   [f"xs{s3}"], eng="pool")
                for h in range(2):
                    for j in range(8):
                        kt = h * 8 + j
                        TR(tp[s][h][:, j, :], xs[s3][:, kt * 128:(kt + 1) * 128], [f"xs{s3}"], [f"tp{s}{h}"])
                    TT(hT[:, h * 8:(h + 1) * 8, tt * 128:(tt + 1) * 128], tp[s][h][:],
                       g1T[:, h * 8:(h + 1) * 8].unsqueeze(2).to_broadcast([128, 8, 128]), ALU.mult,
                       [f"tp{s}{h}"], ["hT"])

            for tt in range(4):
                L1(tt)
            A1(0)
            A1(1)
            A1(2)
            B1(0)
            B1(1)
            for tt in range(NTE):
                if tt == 6:
                    LOADQKV(0)
                if tt + 4 < NTE:
                    L1(tt + 4)
                if tt + 3 < NTE:
                    A1(tt + 3)
                if tt + 2 < NTE:
                    B1(tt + 2)
                C1(tt)
            P.barrier()
            P.flush()

        aout = at("aout", [128, NTT, DA], BF, 80 * K)
        set_cur(112 * K)
        set_cur(136 * K)
        QT = loc("QT", [128, 2, T], BF)
        KTt = loc("KTt", [128, 2, TE], BF)
        Vx = loc("Vx", [128, NTE, 4, VW], BF)
        biasS = loc("biasS", [128, 4, 640], F32)
        sb = [loc(f"sb{i}", [128, 640], F32) for i in range(3)]
        PT = [loc(f"PT{i}", [128, 640], BF) for i in range(3)]
        junk2 = loc("junk2", [128, DA], BF)
        wC = at("wC", [128, KT, 256], BF, 190 * K)
        with ExitStack() as ph:
            pj = [ph.enter_context(nc.psum_tensor(f"pj{i}", [128, 512], F32)) for i in range(2)]
            sA = [ph.enter_context(nc.psum_tensor(f"sA{i}", [128, 512], F32)) for i in range(2)]
            sB = [ph.enter_context(nc.psum_tensor(f"sB{i}", [128, 512], F32)) for i in range(2)]
            oP = [ph.enter_context(nc.psum_tensor(f"oP{i}", [128, 512], F32)) for i in range(2)]
            sA3 = [sA[0], sA[1], pj[0]]
            sB3 = [sB[0], sB[1], pj[1]]
            kA3 = ["sA0", "sA1", "pj0"]
            kB3 = ["sB0", "sB1", "pj1"]
            for h4 in range(4):
                CP("dve", Vx[:, :, h4, 64], valid[:, :], [], ["Vx1"])
            pjc = [0]

            def proj_fm(w, dst, ntc, tok0, wkey, dkey):
                for f2 in range(2):
                    for tc in range(ntc):
                        k = pjc[0] % 2
                        pjc[0] += 1
                        for kt in range(KT):
                            MM(pj[k][:, :], w[:, kt, f2 * 128:(f2 + 1) * 128],
                               hT[:, kt, tok0 + tc * 512: tok0 + (tc + 1) * 512],
                               kt == 0, kt == KT - 1, [wkey], [f"pj{k}"])
                        CP("act", dst[:, f2, tc * 512:(tc + 1) * 512], pj[k][:, :], [f"pj{k}"], [dkey])

            for g in range(4):
                DMA("sp", biasS[:], biasT_d[:, 4 * g:4 * g + 4, :], "bias", [], ["bias"])
                proj_fm(wq, QT, 4, HALO, "wq", "QT")
                proj_fm(wk, KTt, 5, 0, "wk", "KT")
                for tt in range(NTE):
                    k = pjc[0] % 2
                    pjc[0] += 1
                    for kt in range(KT):
                        MM(pj[k][:, 0:256], hT[:, kt, tt * 128:(tt + 1) * 128], wv[:, kt, :],
                           kt == 0, kt == KT - 1, ["wv"], [f"pj{k}"])
                    CP("dve", Vx[:, tt, :, 0:64], pj[k][:, 0:256].rearrange("p (h d) -> p h d", h=4),
                       [f"pj{k}"], ["Vx"])
                if g + 1 < 4:
                    LOADQKV(g + 1)
                else:
                    DMA("pool", wC[:], wblk(4096), "wC", [], ["wC"])
                emit_conv(13 if g == 0 else 9)
                items = [(h4, qt) for h4 in range(4) for qt in range(NTT)]
                n = len(items)

                def S_(i):
                    h4, qt = items[i]
                    s = i % 3
                    p0 = (h4 % 2) * 64
                    f2 = h4 // 2
                    for r in range(5):
                        o = sA3[s][:, r * 128:(r + 1) * 128] if r < 4 else sB3[s][:, 0:128]
                        MM(o, KTt[p0:p0 + 64, f2, (qt + r) * 128:(qt + r + 1) * 128],
                           QT[p0:p0 + 64, f2, qt * 128:(qt + 1) * 128], True, True,
                           ["KT", "QT"], [kA3[s] if r < 4 else kB3[s]])

                def B_(i):
                    h4, qt = items[i]
                    s = i % 3
                    STT(sb[s][:, 0:512], sA3[s][:, :], 0.125, biasS[:, h4, 0:512], ALU.mult, ALU.add,
                        [kA3[s], "bias"], [f"sb{s}a"])
                    STT(sb[s][:, 512:640], sB3[s][:, 0:128], 0.125, biasS[:, h4, 512:640], ALU.mult, ALU.add,
                        [kB3[s], "bias"], [f"sb{s}b"])

                def E_(i):
                    s = i % 3
                    ACTF(PT[s][:, :], sb[s][:, :], AF.Exp, [f"sb{s}a", f"sb{s}b"], [f"PT{s}"])

                def O_(i):
                    h4, qt = items[i]
                    s = i % 2
                    s3 = i % 3
                    for r in range(5):
                        MM(oP[s][:, 0:65], PT[s3][:, r * 128:(r + 1) * 128], Vx[:, qt + r, h4, 0:65],
                           r == 0, r == 4, [f"PT{s3}", "Vx", "Vx1"], [f"oP{s}"])

                def N_(i):
                    h4, qt = items[i]
                    s = i % 2
                    h = g * 4 + h4
                    RCP(rden[s][:, :], oP[s][:, 64:65], [f"oP{s}"], [f"rden{s}"])
                    ACTF(aout[:, qt, h * 64:(h + 1) * 64], oP[s][:, 0:64], AF.Copy, [f"oP{s}", f"rden{s}"],
                         ["aout"], scale=rden[s][:, 0:1])

                S_(0)
                S_(1)
                S_(2)
                B_(0)
                E_(0)
                B_(1)
                E_(1)
                for i in range(n):
                    if i + 2 < n:
                        B_(i + 2)
                    O_(i)
                    if i + 2 < n:
                        E_(i + 2)
                    if i + 3 < n:
                        S_(i + 3)
                    N_(i)
            P.barrier()
            P.flush()

        zc = at("zc", [128, 8, T + 2], BF, 112 * K)
        set_cur(146 * K)
        wU = loc("wU", [128, KT, 256], BF)
        wB = loc("wB", [128, KT, 256], BF)
        yb = [loc(f"yb{i}", [128, 512], F32) for i in range(2)]
        cc = [loc(f"cc{i}", [128, 512], F32) for i in range(2)]
        csq = [loc(f"csq{i}", [128, 512], BF) for i in range(2)]
        zt = loc("zt", [128, D], BF)
        junk3 = loc("junk3", [128, DA], BF)
        with ExitStack() as ph:
            pc = [ph.enter_context(nc.psum_tensor(f"pc{i}", [128, 512], F32)) for i in range(4)]
            css = ph.enter_context(nc.psum_tensor("css", [128, 512], F32))
            pcc = [0]
            first_css = [True]

            def fm_chunk(w, f2, lo, nn, wkey):
                k = pcc[0] % 4
                pcc[0] += 1
                for kt in range(KT):
                    MM(pc[k][:, 0:nn], w[:, kt, f2 * 128:(f2 + 1) * 128], hT[:, kt, lo:lo + nn],
                       kt == 0, kt == KT - 1, [wkey], [f"pc{k}"])
                return k

            def LOADW3(w, c0, j, key):
                DMA("pool", w[:], wblk(c0 + j * 256), key, [], [key])

            P.op("dve", lambda e: e.memset(zt[:], 0.0), [], ["zt"])
            for r0 in range(0, NSLOT, 1024):
                DMA("sp", Xs[r0:r0 + 1024, :].rearrange("(k p) c -> p k c", p=128),
                    zt[:, :].unsqueeze(1).to_broadcast([128, 8, D]), f"xz{(r0 // 1024) % 2}", ["zt"], [])
            DMA("sp", Ys[NSLOT:NSLOT + 128, :], zt[:, :], "yz", ["zt"], [])
            LOADW3(wU, 5120, 0, "wU")
            LOADW3(wB, 3072, 0, "wB")
            sq_todo = list(range(NTT))
            sc_todo = list(range(NTT))
            an_state = {"chain": False}

            def an_act(n):
                for _ in range(n):
                    if sq_todo:
                        tt_ = sq_todo.pop(0)
                        ACTF(junk3[:, :], aout[:, tt_, :], AF.Square, ["aout"], ["junk3", "ssa"],
                             accum_out=ssa[:, tt_:tt_ + 1])
                if not sq_todo and not an_state["chain"]:
                    an_state["chain"] = True
                    rstd_chain(ssa[:, :], None, tmp16[:, :], rstd_a[:, :], NTT, 1.0 / DA, ["ssa"], "rstd_a")

            def an_dve(n):
                if not an_state["chain"]:
                    return
                for _ in range(n):
                    if sc_todo:
                        tt_ = sc_todo.pop(0)
                        TS(aout[:, tt_, :], aout[:, tt_, :], rstd_a[:, tt_:tt_ + 1], 0.0, ALU.mult, ALU.add,
                           ["aout", "rstd_a"], ["aout"])

            for j in range(4):
                zk = lambda ft, tc: f"zc{ft}_{tc}"
                for f2 in range(2):
                    ft = 2 * j + f2
                    k = fm_chunk(wC, f2, HALO - 2, 2, "wC")
                    CP("act", zc[:, ft, 0:2], pc[k][:, 0:2], [f"pc{k}"], [zk(ft, -1)])
                    for tc in range(4):
                        k = fm_chunk(wC, f2, HALO + tc * 512, 512, "wC")
                        CP("act", zc[:, ft, 2 + tc * 512:2 + (tc + 1) * 512], pc[k][:, :], [f"pc{k}"], [zk(ft, tc)])
                        an_act(2)
                if j + 1 < 4:
                    LOADW3(wC, 4096, j + 1, "wC")
                for f2 in range(2):
                    ft = 2 * j + f2
                    k = fm_chunk(wU, f2, HALO - 2, 2, "wU")
                    TT(zc[:, ft, 0:2], pc[k][:, 0:2], zc[:, ft, 0:2], ALU.mult, [f"pc{k}", zk(ft, -1)], [zk(ft, -1)])
                    for tc in range(4):
                        k = fm_chunk(wU, f2, HALO + tc * 512, 512, "wU")
                        sl_ = zc[:, ft, 2 + tc * 512:2 + (tc + 1) * 512]
                        TT(sl_, pc[k][:, :], sl_, ALU.mult, [f"pc{k}", zk(ft, tc)], [zk(ft, tc)])
                        an_dve(2)
                if j + 1 < 4:
                    LOADW3(wU, 5120, j + 1, "wU")
                for f2 in range(2):
                    ft = 2 * j + f2
                    for tc in (3, 2, 1, 0):
                        k = fm_chunk(wB, f2, HALO + tc * 512, 512, "wB")
                        s = tc % 2
                        b0 = tc * 512
                        TS(yb[s][:, :], zc[:, ft, b0 + 2:b0 + 514], cwT[:, ft, 2:3], 0.0, ALU.mult, ALU.add,
                           [zk(ft, tc)], [f"yb{s}"])
                        STT(yb[s][:, :], zc[:, ft, b0 + 1:b0 + 513], cwT[:, ft, 1:2], yb[s][:, :], ALU.mult, ALU.add,
                            [zk(ft, tc), zk(ft, tc - 1), f"yb{s}"], [f"yb{s}"])
                        STT(yb[s][:, :], zc[:, ft, b0:b0 + 512], cwT[:, ft, 0:1], yb[s][:, :], ALU.mult, ALU.add,
                            [zk(ft, tc), zk(ft, tc - 1), f"yb{s}"], [f"yb{s}"])
                        TT(cc[s][:, :], pc[k][:, :], yb[s][:, :], ALU.mult, [f"pc{k}", f"yb{s}"], [f"cc{s}"])
                        ACTF(csq[s][:, :], cc[s][:, :], AF.Square, [f"cc{s}"], [f"csq{s}"])
                        ACTF(zc[:, ft, b0 + 2:b0 + 514], cc[s][:, :], AF.Copy, [f"cc{s}"], [zk(ft, tc)],
                             scale=gcT[:, ft:ft + 1])
                        for i in range(4):
                            tt = tc * 4 + i
                            MM(css[:, tt:tt + 1], csq[s][:, i * 128:(i + 1) * 128], ones[:, 0:1],
                               first_css[0], False, [f"csq{s}"], ["css"], skip=True)
                            first_css[0] = False
                if j + 1 < 4:
                    LOADW3(wB, 3072, j + 1, "wB")
                emit_conv(3)
            an_act(NTT)
            an_dve(NTT)
            assert not sq_todo and not sc_todo
            rstd_chain(css[:, 0:NTT], tmp16[:, :], tmp16b[:, :], rstd_c[:, :], NTT, 1.0 / DC, ["css"], "rstd_c")
            P.barrier()
            P.flush()

        aT = at("aT", [128, 8, T], BF, 0)

        Wo = at("Wo", [128, 16, D], BF, 32 * K)
        set_cur(96 * K)
        xin = [loc(f"xin5{i}", [128, D], F32) for i in range(2)]
        set_cur(146 * K)
        x2t = [loc(f"x2t{i}", [128, D], F32) for i in range(2)]
        h2 = [loc(f"h2{i}", [128, D], BF) for i in range(3)]
        g2b = loc("g2b", [128, D], F32)
        h2T = [loc(f"h2T{i}", [128, KT, 128], BF) for i in range(2)]
        junk = loc("junk5", [128, D], BF)
        wrS = loc("wrS", [128, KT, 36], BF)
        with ExitStack() as ph:
            pa = [ph.enter_context(nc.psum_tensor(f"pa{i}", [128, 512], F32)) for i in range(2)]
            pcv = [ph.enter_context(nc.psum_tensor(f"pcv{i}", [128, 512], F32)) for i in range(2)]
            tp2 = [ph.enter_context(nc.psum_tensor(f"tp2{i}", [128, 8, 128], BF)) for i in range(2)]
            pr = ph.enter_context(nc.psum_tensor("pr", [128, 512], F32))
            pr2 = ph.enter_context(nc.psum_tensor("pr2", [128, 512], F32))
            for i in range(3):
                DMA("sp", Wo[:, 4 * i:4 * i + 4, :], wob_r[:, 4 * i:4 * i + 4, :], f"wo{i}", [], [f"wo{i}"])
            DMA("pool", wrS[:], wr_d.rearrange("p (k c) -> p k c", k=KT), "wr", [], ["wr"])
            DMA("sp", g2b[:], g2b_d, "g2b", [], ["g2b"])
            for tt in range(NTT):
                s = tt % 2
                for ft in range(8):
                    TR(tp2[s][:, ft, :], aout[:, tt, ft * 128:(ft + 1) * 128], ["aout"], [f"tp2{s}"])
                TT(aT[:, :, tt * 128:(tt + 1) * 128], tp2[s][:], gaT[:, 0:8].unsqueeze(2).to_broadcast([128, 8, 128]),
                   ALU.mult, [f"tp2{s}"], ["aT"])
            DMA("sp", Wo[:, 12:16, :], wob_r[:, 12:16, :], "wo3", [], ["wo3", "aout"])
            opc = [0]

            def LOADXIN(tt):
                DMA("sp", xin[tt % 2][:], x_ext[HALO + tt * 128:HALO + (tt + 1) * 128, :], f"xin{tt % 2}", [],
                    [f"xin{tt % 2}"] + (["aout"] if tt < 2 else []))

            LOADXIN(0)

            def OUTPROJ(tt, hooks=None):
                s = tt % 2
                hooks = hooks or {}
                if tt + 1 < NTT:
                    LOADXIN(tt + 1)
                for n4 in range(4):
                    if n4 > 0 and (n4 - 1) in hooks:
                        hooks[n4 - 1]()
                    k = opc[0] % 2
                    opc[0] += 1
                    cs = slice(n4 * 512, (n4 + 1) * 512)
                    for ft in range(8):
                        MM(pa[k][:, :], aT[:, ft, tt * 128:(tt + 1) * 128], Wo[:, ft, cs], ft == 0, ft == 7,
                           [f"wo{ft // 4}", "aT"], [f"pa{k}"])
                    for ft in range(8):
                        MM(pcv[k][:, :], zc[:, ft, 2 + tt * 128:2 + (tt + 1) * 128], Wo[:, 8 + ft, cs],
                           ft == 0, ft == 7, [f"wo{2 + ft // 4}"], [f"pcv{k}"])
                    STT(x2t[s][:, cs], pcv[k][:, :], rstd_c[:, tt:tt + 1], xin[s][:, cs], ALU.mult, ALU.add,
                        [f"pcv{k}", f"xin{s}"], [f"x2t{s}_{n4}"])
                    TT(x2t[s][:, cs], x2t[s][:, cs], pa[k][:, :], ALU.add, [f"pa{k}", f"x2t{s}_{n4}"],
                       [f"x2t{s}_{n4}"])
                x2k = [f"x2t{s}_{n4}" for n4 in range(4)]
                DMA("sp", x2s[tt * 128:(tt + 1) * 128, :], x2t[s][:], f"x2o{s}", x2k, [])
                ACTF(junk[:], x2t[s][:], AF.Square, x2k, ["junk", f"ss{s}"], accum_out=st[s][:, 0:1])
                rstd_chain(st[s][:, 0:1], st[s][:, 1:2], st[s][:, 2:3], st[s][:, 3:4], 1, 1.0 / D,
                           [f"ss{s}"], f"rs{s}")
                STT(h2[tt % 3][:], x2t[s][:], st[s][:, 3:4], g2b[:], ALU.mult, ALU.mult, x2k + [f"rs{s}", "g2b"],
                    [f"h2_{tt % 3}"])

            def TRA(tt):
                s = tt % 2
                s3 = tt % 3
                for h in range(2):
                    for j in range(8):
                        kt = h * 8 + j
                        TR(tp2[h][:, j, :], h2[s3][:, kt * 128:(kt + 1) * 128], [f"h2_{s3}"], [f"tp2{h}"])
                    CP("act", h2T[s][:, h * 8:(h + 1) * 8, :], tp2[h][:], [f"tp2{h}"], [f"h2T{s}{h}"])

            def RA(tt):
                s = tt % 2
                for kt in range(KT):
                    MM(pr[:, 0:36], h2T[s][:, kt, :], wrS[:, kt, :], kt == 0, kt == KT - 1,
                       [f"h2T{s}0", f"h2T{s}1", "wr"], ["pr"])
                TT(lg[:, :], pr[:, 0:36], brb[:, :], ALU.add, ["pr"], ["lg"])
                RED(gmx[:, 0:1], lg[:, 0:4], ALU.max, ["lg"], ["gmax"])
                TT(ohg[:, :], lg[:, 0:4], gmx[:, 0:1].to_broadcast([128, 4]), ALU.is_equal, ["lg", "gmax"], ["ohg"])
                TS(gmx[:, 1:2], gmx[:, 0:1], -1.0, 0.0, ALU.mult, ALU.add, ["gmax"], ["ngmax"])
                ACTF(eg[:, :], lg[:, 0:4], AF.Exp, ["lg", "ngmax"], ["eg", "gsum"], bias=gmx[:, 1:2],
                     accum_out=gmx[:, 2:3])
                TS(pen[:, :], ohg[:, :], 30000.0, -30000.0, ALU.mult, ALU.add, ["ohg"], ["pen"])
                TT(elm[:, :].rearrange("p (g e) -> p g e", g=4), lg[:, 4:36].rearrange("p (g e) -> p g e", g=4),
                   pen[:, 0:4].unsqueeze(2).to_broadcast([128, 4, 8]), ALU.add, ["lg", "pen"], ["elm"])
                P.op("dve", lambda e: e.max(out=m8[:, :], in_=elm[:, :]), ["elm"], ["m8"])
                TT(oh[:, :, :], elm[:, :].unsqueeze(1).to_broadcast([128, 2, NE]),
                   m8[:, 0:2].unsqueeze(2).to_broadcast([128, 2, NE]), ALU.is_equal, ["elm", "m8"], ["oh"])
                TT(Mb[:, :], oh[:, 0, :], oh[:, 1, :], ALU.add, ["oh"], ["Mb"])
                TT(gsc[:, 0:1], m8[:, 1:2], m8[:, 0:1], ALU.subtract, ["m8"], ["gd"])
                ACTF(gsc[:, 1:2], gsc[:, 0:1], AF.Exp, ["gd"], ["ged"])
                TS(gsc[:, 2:3], gsc[:, 1:2], 1.0, 0.0, ALU.add, ALU.add, ["ged"], ["gden"])
                TT(gsc[:, 3:4], gsc[:, 2:3], gmx[:, 2:3], ALU.mult, ["gden", "gsum"], ["gdg"])
                RCP(gw[:, tt, 0:1], gsc[:, 3:4], ["gdg"], [f"gw{tt}a"])
                TT(gw[:, tt, 1:2], gw[:, tt, 0:1], gsc[:, 1:2], ALU.mult, [f"gw{tt}a", "ged"], [f"gw{tt}b"])

            def RB(tt):
                s3 = tt % 3
                MM(pr2[:, 64:96], tri[:, :], Mb[:, :], True, True, ["Mb"], ["pr2"])
                MM(pr2[:, 128:160], ones[:, :], Mb[:, :], True, True, ["Mb"], ["pr2"])
                TT(rk[:, :], pr2[:, 64:96], cnt[:, :], ALU.add, ["pr2", "cnt"], ["rk"])
                TT(cnt[:, :], cnt[:, :], pr2[:, 128:160], ALU.add, ["pr2", "cnt"], ["cnt"])
                TT(tmpe[:, :], rk[:, :], ecap[:, :], ALU.add, ["rk"], ["tmpe"])
                TT(prod[:, :, :], oh[:, :, :], tmpe[:, :].unsqueeze(1).to_broadcast([128, 2, NE]), ALU.mult,
                   ["oh", "tmpe"], ["prod"])
                RED(slf[:, :], prod[:, :, :], ALU.add, ["prod"], ["slf"])
                TT(prod[:, :, :], oh[:, :, :], rk[:, :].unsqueeze(1).to_broadcast([128, 2, NE]), ALU.mult,
                   ["oh", "rk", "slf"], ["prod"])
                RED(rr[:, :], prod[:, :, :], ALU.add, ["prod"], ["rr"])
                TS(tov[:, :], rr[:, :], -float(CAP - 1), 0.0, ALU.add, ALU.max, ["rr"], ["tov"])
                TS(tov[:, :], tov[:, :], 1.0, 1.0, ALU.min, ALU.mult, ["tov"], ["tov"])
                TS(dsl[:, :], slf[:, :], -1.0, float(NSLOT), ALU.mult, ALU.add, ["slf"], ["dsl"])
                TT(dsl[:, :], dsl[:, :], tov[:, :], ALU.mult, ["dsl", "tov"], ["dsl"])
                TT(slf[:, :], slf[:, :], dsl[:, :], ALU.add, ["slf", "dsl"], ["slf"])
                CP("dve", sl_i[:, tt, :], slf[:, :], ["slf"], [f"sl{tt}"])
                if 1 <= tt <= 9:
                    emit_conv(1)
                for kk in range(2):
                    P.dma("pool", lambda e, kk=kk, s3=s3, tt=tt: e.indirect_dma_start(
                        out=Xs[:, :], out_offset=bass.IndirectOffsetOnAxis(ap=sl_i[:, tt, kk:kk + 1], axis=0),
                        in_=h2[s3][:, :], in_offset=None, bounds_check=NSLOT - 1, oob_is_err=False),
                        f"sc{s3}{kk}", [f"h2_{s3}", f"sl{tt}"], [])

            OUTPROJ(0)
            OUTPROJ(1, hooks={1: (lambda: TRA(0)), 2: (lambda: RA(0))})
            for tt in range(NTT):
                hk = {0: (lambda t=tt: RB(t))}
                if tt + 1 < NTT:
                    hk[1] = (lambda t=tt + 1: TRA(t))
                    hk[2] = (lambda t=tt + 1: RA(t))
                if tt + 2 < NTT:
                    OUTPROJ(tt + 2, hooks=hk)
                else:
                    for kk_ in (0, 1, 2):
                        if kk_ in hk:
                            hk[kk_]()
            set_cur(0)
            wg = [loc(f"wg{i}", [128, KT, DE], BF) for i in range(2)]
            wu = [loc(f"wu{i}", [128, KT, DE], BF) for i in range(2)]
            wd = [loc(f"wd{i}", [128, 4, D], BF) for i in range(2)]
            pk_ = ["aT", "wo0", "wo1", "wo2", "wo3"]
            DMA("sp", wg[0][:], wgb[0].rearrange("p (k c) -> p k c", k=KT), "wg0", [], ["wg0"] + pk_)
            DMA("sp", wu[0][:], wub[0].rearrange("p (k c) -> p k c", k=KT), "wu0", [], ["wu0"] + pk_)
            DMA("sp", wd[0][:], wdb[0].rearrange("p (j c) -> p j c", j=4), "wd0", [], ["wd0"] + pk_)
            if dbg is not None:
                P.op("dve", lambda e: e.tensor_copy(out=x2t[0][:, 0:NTT * 8].rearrange("p (t k) -> p t k", k=8)[:, :, 0:2],
                                                    in_=sl_i[:, :, :]), [], ["dbgt"] + [f"x2t0_{n4}" for n4 in range(4)])
                P.op("dve", lambda e: e.tensor_copy(out=x2t[0][:, 0:NTT * 8].rearrange("p (t k) -> p t k", k=8)[:, :, 2:4],
                                                    in_=gw[:, :, :]), ["dbgt"], ["dbgt"])
                DMA("sp", dbg, x2t[0][:, 0:NTT * 8].rearrange("p (t k) -> p t k", k=8), "dbg", ["dbgt"], [])
            P.barrier()
            P.flush()

        if stage >= 6:
            set_cur(96 * K)
            xr = [loc(f"xr{i}", [128, NSUB, D], BF) for i in range(2)]
            xbT = [loc(f"xbT{i}", [128, KT, CAP], BF) for i in range(2)]
            aTe = [loc(f"aTe{i}", [128, 4, CAP], BF) for i in range(2)]
            sg = [loc(f"sg{i}", [128, CAP], F32) for i in range(2)]
            ysb = [loc(f"ysb{i}", [128, D], BF) for i in range(4)]
            with ExitStack() as ph:
                tpe = [ph.enter_context(nc.psum_tensor(f"tpe{i}", [128, 8, 128], BF)) for i in range(2)]
                pg_ = [ph.enter_context(nc.psum_tensor(f"pg{i}", [128, 512], F32)) for i in range(2)]
                pu_ = [ph.enter_context(nc.psum_tensor(f"pu{i}", [128, 512], F32)) for i in range(2)]
                py = [ph.enter_context(nc.psum_tensor(f"py{i}", [128, 512], F32)) for i in range(2)]
                c1 = [0, 0, 0, 0]

                def LOADW(e_):
                    s = e_ % 2
                    if e_ < NCONV:
                        DMA("sp", wg[s][:], wgb[e_].rearrange("p (k c) -> p k c", k=KT), f"wg{s}", [], [f"wg{s}"])
                        DMA("sp", wu[s][:], wub[e_].rearrange("p (k c) -> p k c", k=KT), f"wu{s}", [], [f"wu{s}"])
                        DMA("sp", wd[s][:], wdb[e_].rearrange("p (j c) -> p j c", j=4), f"wd{s}", [], [f"wd{s}"])
                    else:
                        DMA("pool", wg[s][:], w_gate[e_].rearrange("p (k c) -> p k c", k=KT), f"wgp{s}", [], [f"wg{s}"])
                        DMA("pool", wu[s][:], w_up[e_].rearrange("p (k c) -> p k c", k=KT), f"wup{s}", [], [f"wu{s}"])
                        DMA("pool", wd[s][:], w_down[e_].rearrange("p (j c) -> p j c", j=4), f"wdp{s}", [], [f"wd{s}"])

                def LOADX(e_):
                    s = e_ % 2
                    nf = CAP // 128
                    DMA("sp", xr[s][:, 0:nf, :],
                        Xs[e_ * CAP:e_ * CAP + nf * 128, :].rearrange("(sub p) c -> p sub c", p=128),
                        f"xr{s}", [], [f"xr{s}a"])
                    if CAP % 128:
                        rem = CAP % 128
                        DMA("sp", xr[s][0:rem, nf, :], Xs[e_ * CAP + nf * 128:(e_ + 1) * CAP, :],
                            f"xr{s}b", [], [f"xr{s}b"])

                def TRX(e_):
                    s = e_ % 2
                    for sub in range(NSUB):
                        rs = RSZ[sub]
                        xkey = f"xr{s}a" if rs == 128 else f"xr{s}b"
                        for h in range(2):
                            k = c1[0] % 2
                            c1[0] += 1
                            for j in range(8):
                                kt = h * 8 + j
                                P.op("pe", lambda e, k=k, j=j, s=s, sub=sub, kt=kt, rs=rs: e.transpose(
                                    out=tpe[k][:, j, 0:rs], in_=xr[s][0:rs, sub, kt * 128:(kt + 1) * 128],
                                    identity=ident[0:rs, 0:rs]), [xkey], [f"tpe{k}"])
                            CP("act" if k == 0 else "dve", xbT[s][:, h * 8:(h + 1) * 8, sub * 128:sub * 128 + rs],
                               tpe[k][:, :, 0:rs], [f"tpe{k}"], [f"xbT{s}_{sub}{h}"])

                def GU(e_):
                    s = e_ % 2
                    xk = [f"xbT{s}_{a}{b}" for a in range(NSUB) for b in range(2)]
                    for jt in range(4):
                        k = c1[1] % 2
                        c1[1] += 1
                        for kt in range(KT):
                            MM(pg_[k][:, 0:CAP], wg[s][:, kt, jt * 128:(jt + 1) * 128], xbT[s][:, kt, :],
                               kt == 0, kt == KT - 1, xk + [f"wg{s}"], [f"pg{k}"])
                        for kt in range(KT):
                            MM(pu_[k][:, 0:CAP], wu[s][:, kt, jt * 128:(jt + 1) * 128], xbT[s][:, kt, :],
                               kt == 0, kt == KT - 1, xk + [f"wu{s}"], [f"pu{k}"])
                        ACTF(sg[k][:, :], pg_[k][:, 0:CAP], AF.Silu, [f"pg{k}"], [f"sg{k}"])
                        TT(aTe[s][:, jt, :], sg[k][:, :], pu_[k][:, 0:CAP], ALU.mult, [f"pu{k}", f"sg{k}"],
                           [f"aTe{s}_{jt}"])

                def YY(e_):
                    s = e_ % 2
                    ak = [f"aTe{s}_{jt}" for jt in range(4)]
                    for sub in range(NSUB):
                        rs = RSZ[sub]
                        ys = c1[2] % 4
                        c1[2] += 1
                        for n4 in range(4):
                            k = c1[3] % 2
                            c1[3] += 1
                            for jt in range(4):
                                MM(py[k][0:rs, :], aTe[s][:, jt, sub * 128:sub * 128 + rs],
                                   wd[s][:, jt, n4 * 512:(n4 + 1) * 512], jt == 0, jt == 3,
                                   ak + [f"wd{s}"], [f"py{k}"])
                            CP("act" if n4 % 2 == 0 else "dve", ysb[ys][0:rs, n4 * 512:(n4 + 1) * 512], py[k][0:rs, :],
                               [f"py{k}"], [f"ysb{ys}_{n4}"])
                        r0 = e_ * CAP + sub * 128
                        DMA("pool", Ys[r0:r0 + rs, :], ysb[ys][0:rs, :], f"yo{ys}", [f"ysb{ys}_{n4}" for n4 in range(4)], [])

                LOADX(0)
                TRX(0)
                for e_ in range(NE):
                    if e_ + 1 < NE:
                        LOADW(e_ + 1)
                        LOADX(e_ + 1)
                    GU(e_)
                    if e_ + 1 < NE:
                        TRX(e_ + 1)
                    YY(e_)
                    if e_ == NE - 4:
                        set_cur(165 * K)
                        x2t = [loc(f"x2f{i}", [128, D], F32) for i in range(3)]
                        gfb = loc("gfb", [128, D], F32)
                        DMA("sp", gfb[:], gfb_d, "gfb", [], ["gfb"])
                        for t7 in range(3):
                            DMA("sp", x2t[t7][:], x2s[t7 * 128:(t7 + 1) * 128, :], f"x2f{t7}", [], [f"x2f{t7}"])
                P.barrier()
                P.flush()

            set_cur(0)
            ya = [loc(f"ya{i}", [128, D], BF) for i in range(3)]
            ybb = [loc(f"ybb{i}", [128, D], BF) for i in range(3)]
            x3 = [loc(f"x3{i}", [128, D], F32) for i in range(2)]
            ot = [loc(f"ot{i}", [128, D], F32) for i in range(2)]
            junk = loc("junk7", [128, D], BF)

            def LOAD7(tt):
                s3 = tt % 3
                if tt >= 3:
                    DMA("sp", x2t[s3][:], x2s[tt * 128:(tt + 1) * 128, :], f"x2f{s3}", [], [f"x2f{s3}"])
                for kk, dst in ((0, ya), (1, ybb)):
                    P.dma("pool", lambda e, kk=kk, dst=dst, s3=s3, tt=tt: e.indirect_dma_start(
                        out=dst[s3][:, :], out_offset=None, in_=Ys[:, :],
                        in_offset=bass.IndirectOffsetOnAxis(ap=sl_i[:, tt, kk:kk + 1], axis=0)),
                        f"yg{s3}{kk}", [], [f"yg{s3}{kk}"])

            def X37(tt):
                s = tt % 2
                s3 = tt % 3
                STT(x3[s][:], ya[s3][:], gw[:, tt, 0:1], x2t[s3][:], ALU.mult, ALU.add, [f"yg{s3}0", f"x2f{s3}"],
                    [f"x3{s}"])
                STT(x3[s][:], ybb[s3][:], gw[:, tt, 1:2], x3[s][:], ALU.mult, ALU.add, [f"yg{s3}1", f"x3{s}"], [f"x3{s}"])
                ACTF(junk[:], x3[s][:], AF.Square, [f"x3{s}"], ["junk", f"ss{s}"], accum_out=st[s][:, 0:1])
                ACTF(st[s][:, 2:3], st[s][:, 0:1], AF.Sqrt, [f"ss{s}"], [f"rs{s}_s"], scale=1.0 / D, bias=epsT[:, 0:1])

            def FIN7(tt):
                s = tt % 2
                RCP(st[s][:, 3:4], st[s][:, 2:3], [f"rs{s}_s"], [f"rs{s}"])
                STT(ot[s][:], x3[s][:], st[s][:, 3:4], gfb[:], ALU.mult, ALU.mult, [f"x3{s}", f"rs{s}", "gfb"], [f"ot{s}"])
                DMA("sp", out_d[tt * 128:(tt + 1) * 128, :], ot[s][:], f"oo{s}", [f"ot{s}"], [])

            LOAD7(0)
            LOAD7(1)
            X37(0)
            for tt in range(NTT):
                if tt + 2 < NTT:
                    LOAD7(tt + 2)
                if tt + 1 < NTT:
                    X37(tt + 1)
                FIN7(tt)
            P.barrier()
            P.flush()
    return nc


def _bias_table(rel_bias):
    kk = np.arange(128)[:, None, None]
    r = np.arange(5)[None, :, None]
    qq = np.arange(128)[None, None, :]
    dist = 512 + qq - r * 128 - kk
    idx = np.clip(dist, -256, 256) + 256
    dch = (8 + qq // 64) - (2 * r + kk // 64)
    ok = (dch >= 0) & (dch <= 8)
    tab = rel_bias[:, idx]
    tab = np.where(ok[None], tab, np.float32(-1e30)).astype(np.float32)
    return np.ascontiguousarray(tab.transpose(1, 0, 2, 3).reshape(128, 16, 640))


def kernel(x, norm1, w_in, rel_bias, conv_w, g_out_attn, g_out_conv, w_out, norm2,
           w_router_group, b_router_group, w_router_expert, b_router_expert,
           w_gate, w_up, w_down, norm_final):
    stage = int(os.environ.get("MK_STAGE", "99"))
    f = lambda a: np.ascontiguousarray(np.asarray(a, dtype=np.float32))
    x = f(x)
    B, S, _ = x.shape
    shared = dict(
        w_in=f(np.asarray(w_in[0]).reshape(KT, 128, 24, 256).transpose(2, 1, 0, 3)).reshape(24, 128, KT * 256),
        g1T=f(np.asarray(norm1[0]).reshape(KT, 128).T),
        biasT=_bias_table(f(rel_bias[0])),
        cwT=f(np.asarray(conv_w[0]).reshape(3, 8, 128).transpose(2, 1, 0)),
        gaT=f(np.asarray(g_out_attn[0]).reshape(8, 128).T),
        gcT=f(np.asarray(g_out_conv[0]).reshape(8, 128).T),
        w_out=f(np.asarray(w_out[0]).reshape(KT, 128, D).transpose(1, 0, 2)).reshape(128, KT * D),
        g2b=f(np.broadcast_to(np.asarray(norm2[0])[None, :], (128, D))),
        gfb=f(np.broadcast_to(np.asarray(norm_final)[None, :], (128, D))),
        wr=f(np.concatenate([np.asarray(w_router_group[0]), np.asarray(w_router_expert[0])], axis=1)
             .reshape(KT, 128, 36).transpose(1, 0, 2)).reshape(128, KT * 36),
        brb=f(np.broadcast_to(np.concatenate([np.asarray(b_router_group[0]),
                                              np.asarray(b_router_expert[0])])[None, :], (128, 36))),
        w_gate=f(np.asarray(w_gate[0]).reshape(NE, KT, 128, DE).transpose(0, 2, 1, 3)).reshape(NE, 128, KT * DE),
        w_up=f(np.asarray(w_up[0]).reshape(NE, KT, 128, DE).transpose(0, 2, 1, 3)).reshape(NE, 128, KT * DE),
        w_down=f(np.asarray(w_down[0]).reshape(NE, 4, 128, D).transpose(0, 2, 1, 3)).reshape(NE, 128, 4 * D),
        ident=np.eye(128, dtype=np.float32).astype(bf16),
        tri=np.triu(np.ones((128, 128), np.float32), 1).astype(bf16),
        ones=np.ones((128, 128), np.float32).astype(bf16),
        ecap=f(np.broadcast_to((np.arange(NE) * CAP)[None, :], (128, NE))),
    )
    in_maps = []
    for c in range(NCORES):
        b, half = c // 2, c % 2
        xe = np.zeros((TE, D), np.float32)
        if half == 1:
            xe[:HALO] = x[b, T - HALO:T]
        xe[HALO:] = x[b, half * T:(half + 1) * T]
        val = np.ones((NTE, 128), np.float32)
        if half == 0:
            val[:HALO // 128] = 0.0
        m = dict(shared)
        m["x_ext"] = xe
        m["valid"] = np.ascontiguousarray(val.T)
        in_maps.append(m)
    nc = build(stage)
    res = run_bass_kernel_spmd(nc, in_maps, core_ids=list(range(NCORES)))
    key = "out" if stage >= 99 else "x2s"
    out = np.empty((B, S, D), np.float32)
    for c in range(NCORES):
        b, half = c // 2, c % 2
        out[b, half * T:(half + 1) * T] = res.results[c][key]
    if stage < 99:
        kernel.dbg = [res.results[c]["dbg"] for c in range(NCORES)]
    return out
```

```python
import os
from contextlib import ExitStack

import ml_dtypes
import numpy as np

import concourse.bass as bass
import concourse.mybir as mybir
from concourse.bass_utils import run_bass_kernel_spmd

F32 = mybir.dt.float32
BF = mybir.dt.bfloat16
I32 = mybir.dt.int32
ALU = mybir.AluOpType
AF = mybir.ActivationFunctionType
AX = mybir.AxisListType
bf16 = ml_dtypes.bfloat16

NCORES = 8
D = 2048
KT = 16
T = 2048
HALO = 512
TE = T + HALO
NTT = T // 128
NTE = TE // 128
DA = 1024
DC = 1024
NE = 32
DE = 512
CAP = 320
NSUB = (CAP + 127) // 128
RSZ = [min(128, CAP - 128 * i) for i in range(NSUB)]
NSLOT = NE * CAP
EPS = 1e-6
BIGT = 20000.0
VW = 66

ENGS = ("pe", "dve", "act", "pool", "sp")


class Op:
    __slots__ = ("eng", "emit", "deps", "is_dma", "sem_key", "val", "signal")

    def __init__(self, eng, emit, is_dma, sem_key=None, val=0):
        self.eng = eng
        self.emit = emit
        self.deps = []
        self.is_dma = is_dma
        self.sem_key = sem_key
        self.val = val
        self.signal = False


class Prog:
    def __init__(self, nc, stack):
        self.nc = nc
        self.pending = {e: [] for e in ENGS}
        self.cnt = {e: 0 for e in ENGS}
        self.dma_cnt = {}
        self.sems = {}
        self.waited = {e: {} for e in ENGS}
        self.last_w = {}
        self.readers = {}
        self.last_op = {e: None for e in ENGS}
        self._stack = stack
        self._old_dmas = []

    def _sem(self, name):
        if name not in self.sems:
            self.sems[name] = self._stack.enter_context(self.nc.semaphore(name))
        return self.sems[name]

    def _track(self, op, reads, writes, extra):
        deps = []
        for k in reads:
            w = self.last_w.get(k)
            if w is not None:
                deps.append(w)
        for k in writes:
            w = self.last_w.get(k)
            if w is not None:
                deps.append(w)
            deps.extend(self.readers.get(k, ()))
        deps.extend(extra)
        seen = set()
        for d in deps:
            if d is None or d is op or id(d) in seen:
                continue
            if d.eng == "pe" and op.eng == "pe" and not d.is_dma and not op.is_dma:
                continue
            seen.add(id(d))
            op.deps.append(d)
            if not d.is_dma:
                d.signal = True
        for k in reads:
            self.readers.setdefault(k, []).append(op)
        for k in writes:
            self.last_w[k] = op
            self.readers[k] = []

    def op(self, eng, emit, reads=(), writes=(), deps=()):
        o = Op(eng, emit, False)
        self._track(o, reads, writes, deps)
        self.pending[eng].append(o)
        self.last_op[eng] = o
        return o

    def dma(self, eng, emit, sem_key, reads=(), writes=(), deps=()):
        c = self.dma_cnt.get(sem_key, 0) + 16
        self.dma_cnt[sem_key] = c
        o = Op(eng, emit, True, sem_key="d_" + sem_key, val=c)
        self._track(o, reads, writes, deps)
        self.pending[eng].append(o)
        return o

    def barrier(self):
        lasts = [self.last_op[e] for e in ENGS if self.last_op[e] is not None]
        best = {}
        for o in [o for e in ENGS for o in self.pending[e] if o.is_dma] + self._old_dmas:
            if o.sem_key not in best or best[o.sem_key].val < o.val:
                best[o.sem_key] = o
        alld = lasts + list(best.values())
        for e in ENGS:
            o = Op(e, None, False)
            for d in alld:
                o.deps.append(d)
                if not d.is_dma:
                    d.signal = True
            self.pending[e].append(o)
        self.last_w = {}
        self.readers = {}

    def flush(self):
        nc = self.nc
        for e in ENGS:
            for o in self.pending[e]:
                if not o.is_dma and o.signal:
                    self.cnt[e] += 1
                    o.val = self.cnt[e]
                    o.sem_key = "e_" + e
        for e in ENGS:
            for o in self.pending[e]:
                if o.sem_key is not None:
                    self._sem(o.sem_key)
        pend = self.pending
        self.pending = {e: [] for e in ENGS}
        best = {}
        for o in [o for e in ENGS for o in pend[e] if o.is_dma] + self._old_dmas:
            if o.sem_key not in best or best[o.sem_key].val < o.val:
                best[o.sem_key] = o
        self._old_dmas = list(best.values())

        def run(e, engine):
            waited = self.waited[e]
            for o in pend[e]:
                for d in sorted(o.deps, key=lambda d_: -d_.val):
                    if waited.get(d.sem_key, 0) >= d.val:
                        continue
                    engine.wait_ge(self.sems[d.sem_key], d.val)
                    waited[d.sem_key] = d.val
                if o.emit is None:
                    continue
                inst = o.emit(engine)
                if o.is_dma:
                    inst.then_inc(self.sems[o.sem_key], 16)
                elif o.signal:
                    inst.then_inc(self.sems[o.sem_key], 1)

        with nc.Block() as block:
            if pend["pe"]:
                @block.tensor
                def _(eng):
                    run("pe", eng)
            if pend["dve"]:
                @block.vector
                def _(eng):
                    run("dve", eng)
            if pend["act"]:
                @block.scalar
                def _(eng):
                    run("act", eng)
            if pend["pool"]:
                @block.gpsimd
                def _(eng):
                    run("pool", eng)
            if pend["sp"]:
                @block.sync
                def _(eng):
                    run("sp", eng)


def build(stage=99):
    nc = bass.Bass("TRN2", target_bir_lowering=False)

    def din(name, shape, dt=F32):
        return nc.dram_tensor(name, shape, dt, kind="ExternalInput").ap()

    x_ext = din("x_ext", [TE, D])
    valid_d = din("valid", [128, NTE])
    w_in = din("w_in", [24, 128, KT * 256])
    g1T_d = din("g1T", [128, KT])
    biasT_d = din("biasT", [128, 16, 640])
    cwT_d = din("cwT", [128, 8, 3])
    gaT_d = din("gaT", [128, 8])
    gcT_d = din("gcT", [128, 8])
    w_out = din("w_out", [128, KT * D])
    g2b_d = din("g2b", [128, D])
    gfb_d = din("gfb", [128, D])
    wr_d = din("wr", [128, KT * 36])
    brb_d = din("brb", [128, 36])
    w_gate = din("w_gate", [NE, 128, KT * DE])
    w_up = din("w_up", [NE, 128, KT * DE])
    w_down = din("w_down", [NE, 128, 4 * D])
    ident_d = din("ident", [128, 128], BF)
    tri_d = din("tri", [128, 128], BF)
    ones_d = din("ones", [128, 128], BF)
    ecap_d = din("ecap", [128, NE])
    out_d = nc.dram_tensor("out", [T, D], F32, kind="ExternalOutput").ap()
    if stage < 99:
        x2s = nc.dram_tensor("x2s", [T, D], F32, kind="ExternalOutput").ap()
        dbg = nc.dram_tensor("dbg", [128, NTT, 8], F32, kind="ExternalOutput").ap()
    else:
        x2s = nc.dram_tensor("x2s", [T, D], F32).ap()
        dbg = None
    NCONV = 19
    wgb = nc.dram_tensor("wgb", [NCONV, 128, KT * DE], BF).ap()
    wub = nc.dram_tensor("wub", [NCONV, 128, KT * DE], BF).ap()
    wdb = nc.dram_tensor("wdb", [NCONV, 128, 4 * D], BF).ap()
    wob = nc.dram_tensor("wob", [128, KT * D], BF).ap()
    Xs = nc.dram_tensor("Xs", [NSLOT, D], BF).ap()
    Ys = nc.dram_tensor("Ys", [NSLOT + 128, D], BF).ap()

    def wblk(c0):
        return w_in[c0 // 256].rearrange("p (k c) -> p k c", k=KT)

    w_out_r = w_out.rearrange("p (f c) -> p f c", f=KT)
    wob_r = wob.rearrange("p (f c) -> p f c", f=KT)

    BASE = 16640
    TOP = 229376
    SMALL = TOP - 6144
    esz = {F32: 4, BF: 2, I32: 4}
    cur = [BASE]
    uid = [0]

    def at(name, shape, dt, off):
        uid[0] += 1
        n = 1
        for s in shape[1:]:
            n *= s
        assert BASE + off + n * esz[dt] <= TOP, (name, off, n * esz[dt])
        return nc.alloc_sbuf_tensor_at(f"{name}_{uid[0]}", list(shape), dt, offset=BASE + off)

    def loc(name, shape, dt, limit=SMALL):
        n = 1
        for s in shape[1:]:
            n *= s
        sz = (n * esz[dt] + 31) // 32 * 32
        off = cur[0]
        assert off + sz <= limit, (name, off, sz, limit)
        cur[0] = off + sz
        uid[0] += 1
        return nc.alloc_sbuf_tensor_at(f"{name}_{uid[0]}", list(shape), dt, offset=off)

    def set_cur(off):
        cur[0] = BASE + off

    K = 1024

    with ExitStack() as top:
        P = Prog(nc, top)

        def MM(out, lhsT, rhs, start, stop, reads, writes, skip=False):
            if skip:
                P.op("pe", lambda e: e.matmul(out, lhsT=lhsT, rhs=rhs, start=start, stop=stop,
                                              skip_group_check=True), reads, writes)
            else:
                P.op("pe", lambda e: e.matmul(out, lhsT=lhsT, rhs=rhs, start=start, stop=stop),
                     reads, writes)

        def TR(out, in_, reads, writes):
            P.op("pe", lambda e: e.transpose(out=out, in_=in_, identity=ident[:]), reads, writes)

        def ACTF(out, in_, func, reads, writes, **kw):
            P.op("act", lambda e: e.activation(out=out, in_=in_, func=func, **kw), reads, writes)

        def TS(out, in0, s1, s2, op0, op1, reads, writes, eng="dve"):
            P.op(eng, lambda e: e.tensor_scalar(out=out, in0=in0, scalar1=s1, scalar2=s2, op0=op0, op1=op1),
                 reads, writes)

        def STT(out, in0, scalar, in1, op0, op1, reads, writes, eng="dve"):
            P.op(eng, lambda e: e.scalar_tensor_tensor(out=out, in0=in0, scalar=scalar, in1=in1,
                                                       op0=op0, op1=op1), reads, writes)

        def TT(out, in0, in1, op, reads, writes, eng="dve"):
            P.op(eng, lambda e: e.tensor_tensor(out=out, in0=in0, in1=in1, op=op), reads, writes)

        def CP(eng, out, in_, reads, writes):
            if eng == "act":
                P.op("act", lambda e: e.copy(out=out, in_=in_), reads, writes)
            else:
                P.op(eng, lambda e: e.tensor_copy(out=out, in_=in_), reads, writes)

        def RED(out, in_, op, reads, writes):
            P.op("dve", lambda e: e.tensor_reduce(out=out, in_=in_, axis=AX.X, op=op), reads, writes)

        def RCP(out, in_, reads, writes):
            P.op("dve", lambda e: e.reciprocal(out=out, in_=in_), reads, writes)

        def DMA(q, out, in_, key, reads, writes):
            return P.dma(q, lambda e: e.dma_start(out=out, in_=in_), key, reads, writes)

        def rstd_chain(ss, tmp, srt, out, n, inv_n, rk, wk):
            ACTF(srt, ss, AF.Sqrt, rk, [wk + "_s"], scale=inv_n, bias=epsT[:, 0:1])
            RCP(out, srt, [wk + "_s"], [wk])

        conv_list = []
        for q_ in range(4):
            conv_list.append((wob[:, q_ * 4 * D:(q_ + 1) * 4 * D], w_out[:, q_ * 4 * D:(q_ + 1) * 4 * D]))
        for e_ in range(NCONV):
            conv_list.append((wgb[e_], w_gate[e_]))
            conv_list.append((wub[e_], w_up[e_]))
            conv_list.append((wdb[e_], w_down[e_]))
        conv_pos = [0]

        def emit_conv(n):
            for _ in range(n):
                if conv_pos[0] >= len(conv_list):
                    return
                o_, i_ = conv_list[conv_pos[0]]
                DMA("pool", o_, i_, f"cv{conv_pos[0] % 4}", [], [])
                conv_pos[0] += 1

        cur[0] = SMALL
        LIM = TOP
        ident = loc("ident", [128, 128], BF, LIM)
        tri = loc("tri", [128, 128], BF, LIM)
        ones = loc("ones", [128, 128], BF, LIM)
        g1T = loc("g1T", [128, KT], F32, LIM)
        gaT = loc("gaT", [128, 8], F32, LIM)
        gcT = loc("gcT", [128, 8], F32, LIM)
        cwT = loc("cwT", [128, 8, 3], F32, LIM)
        valid = loc("valid", [128, NTE], F32, LIM)
        ecap = loc("ecap", [128, NE], F32, LIM)
        brb = loc("brb", [128, 36], F32, LIM)
        sl_i = loc("sl_i", [128, NTT, 2], I32, LIM)
        gw = loc("gw", [128, NTT, 2], F32, LIM)
        rstd_c = loc("rstd_c", [128, NTT], F32, LIM)
        rstd_a = loc("rstd_a", [128, NTT], F32, LIM)
        ssa = loc("ssa", [128, NTT], F32, LIM)
        tmp16 = loc("tmp16", [128, NTT], F32, LIM)
        tmp16b = loc("tmp16b", [128, NTT], F32, LIM)
        cnt = loc("cnt", [128, NE], F32, LIM)
        epsT = loc("epsT", [128, 1], F32, LIM)
        st6 = [loc(f"st6{i}", [128, 4], F32, LIM) for i in range(6)]
        st = [loc(f"st{i}", [128, 4], F32, LIM) for i in range(2)]
        rden = [loc(f"rden{i}", [128, 1], F32, LIM) for i in range(2)]
        lg = loc("lg", [128, 36], F32, LIM)
        gmx = loc("gmx", [128, 4], F32, LIM)
        ohg = loc("ohg", [128, 4], F32, LIM)
        pen = loc("pen", [128, 4], F32, LIM)
        eg = loc("eg", [128, 4], F32, LIM)
        elm = loc("elm", [128, NE], F32, LIM)
        m8 = loc("m8", [128, 8], F32, LIM)
        oh = loc("oh", [128, 2, NE], F32, LIM)
        prod = loc("prod", [128, 2, NE], F32, LIM)
        Mb = loc("Mb", [128, NE], BF, LIM)
        rk = loc("rk", [128, NE], F32, LIM)
        tmpe = loc("tmpe", [128, NE], F32, LIM)
        slf = loc("slf", [128, 2], F32, LIM)
        rr = loc("rr", [128, 2], F32, LIM)
        tov = loc("tov", [128, 2], F32, LIM)
        dsl = loc("dsl", [128, 2], F32, LIM)
        gsc = loc("gsc", [128, 4], F32, LIM)

        for (t_, d_, k_) in ((ident, ident_d, "c0"), (tri, tri_d, "c1"), (ones, ones_d, "c2"), (g1T, g1T_d, "c3"),
                             (gaT, gaT_d, "c4"), (gcT, gcT_d, "c5"), (cwT, cwT_d, "c6"), (valid, valid_d, "c7"),
                             (ecap, ecap_d, "c8"), (brb, brb_d, "c9")):
            DMA("sp", t_[:], d_, k_, [], [k_])
        P.op("dve", lambda e: e.memset(cnt[:], 0.0), [], ["cnt"])
        P.op("dve", lambda e: e.memset(epsT[:], EPS), [], ["epsT"])
        P.barrier()

        hT = at("hT", [128, KT, TE], BF, 0)
        set_cur(112 * K)
        wq = loc("wq", [128, KT, 256], BF)
        wk = loc("wk", [128, KT, 256], BF)
        wv = loc("wv", [128, KT, 256], BF)

        def LOADQKV(g):
            DMA("pool", wq[:], wblk(g * 256), "wq", [], ["wq"])
            DMA("pool", wk[:], wblk(1024 + g * 256), "wk", [], ["wk"])
            DMA("pool", wv[:], wblk(2048 + g * 256), "wv", [], ["wv"])

        set_cur(137 * K)
        xin = [loc(f"xin{i}", [128, D], F32) for i in range(6)]
        xs = [loc(f"xs{i}", [128, D], BF) for i in range(3)]
        junk = loc("junk", [128, D], BF)
        with ExitStack() as ph:
            tp = [[ph.enter_context(nc.psum_tensor(f"tp{s}{h}", [128, 8, 128], BF)) for h in range(2)]
                  for s in range(2)]

            def L1(tt):
                s6 = tt % 6
                DMA("sp", xin[s6][:], x_ext[tt * 128:(tt + 1) * 128, :], f"xin{s6}", [], [f"xin{s6}"])

            def A1(tt):
                s6 = tt % 6
                ACTF(junk[:], xin[s6][:], AF.Square, [f"xin{s6}"], ["junk", f"ss{s6}"], accum_out=st6[s6][:, 0:1])

            def B1(tt):
                s6 = tt % 6
                rstd_chain(st6[s6][:, 0:1], None, st6[s6][:, 2:3], st6[s6][:, 3:4], 1, 1.0 / D, [f"ss{s6}"], f"rs{s6}")

            def C1(tt):
                s6 = tt % 6
                s3 = tt % 3
                s = tt % 2
                TS(xs[s3][:], xin[s6][:], st6[s6][:, 3:4], 0.0, ALU.mult, ALU.add, [f"xin{s6}", f"rs{s6}"],
                   [f"xs{s3}"], eng="pool")
                for h in range(2):
                    for j in range(8):
                        kt = h * 8 + j
                        TR(tp[s][h][:, j, :], xs[s3][:, kt * 128:(kt + 1) * 128], [f"xs{s3}"], [f"tp{s}{h}"])
                    TT(hT[:, h * 8:(h + 1) * 8, tt * 128:(tt + 1) * 128], tp[s][h][:],
                       g1T[:, h * 8:(h + 1) * 8].unsqueeze(2).to_broadcast([128, 8, 128]), ALU.mult,
                       [f"tp{s}{h}"], ["hT"])

            for tt in range(4):
                L1(tt)
            A1(0)
            A1(1)
            A1(2)
            B1(0)
            B1(1)
            for tt in range(NTE):
                if tt == 6:
                    LOADQKV(0)
                if tt + 4 < NTE:
                    L1(tt + 4)
                if tt + 3 < NTE:
                    A1(tt + 3)
                if tt + 2 < NTE:
                    B1(tt + 2)
                C1(tt)
            P.barrier()
            P.flush()

        aout = at("aout", [128, NTT, DA], BF, 80 * K)
        set_cur(112 * K)
        set_cur(136 * K)
        QT = loc("QT", [128, 2, T], BF)
        KTt = loc("KTt", [128, 2, TE], BF)
        Vx = loc("Vx", [128, NTE, 4, VW], BF)
        biasS = loc("biasS", [128, 4, 640], F32)
        sb = [loc(f"sb{i}", [128, 640], F32) for i in range(3)]
        PT = [loc(f"PT{i}", [128, 640], BF) for i in range(3)]
        junk2 = loc("junk2", [128, DA], BF)
        wC = at("wC", [128, KT, 256], BF, 190 * K)
        with ExitStack() as ph:
            pj = [ph.enter_context(nc.psum_tensor(f"pj{i}", [128, 512], F32)) for i in range(2)]
            sA = [ph.enter_context(nc.psum_tensor(f"sA{i}", [128, 512], F32)) for i in range(2)]
            sB = [ph.enter_context(nc.psum_tensor(f"sB{i}", [128, 512], F32)) for i in range(2)]
            oP = [ph.enter_context(nc.psum_tensor(f"oP{i}", [128, 512], F32)) for i in range(2)]
            sA3 = [sA[0], sA[1], pj[0]]
            sB3 = [sB[0], sB[1], pj[1]]
            kA3 = ["sA0", "sA1", "pj0"]
            kB3 = ["sB0", "sB1", "pj1"]
            for h4 in range(4):
                CP("dve", Vx[:, :, h4, 64], valid[:, :], [], ["Vx1"])
            pjc = [0]

            def proj_fm(w, dst, ntc, tok0, wkey, dkey):
                for f2 in range(2):
                    for tc in range(ntc):
                        k = pjc[0] % 2
                        pjc[0] += 1
                        for kt in range(KT):
                            MM(pj[k][:, :], w[:, kt, f2 * 128:(f2 + 1) * 128],
                               hT[:, kt, tok0 + tc * 512: tok0 + (tc + 1) * 512],
                               kt == 0, kt == KT - 1, [wkey], [f"pj{k}"])
                        CP("act", dst[:, f2, tc * 512:(tc + 1) * 512], pj[k][:, :], [f"pj{k}"], [dkey])

            for g in range(4):
                DMA("sp", biasS[:], biasT_d[:, 4 * g:4 * g + 4, :], "bias", [], ["bias"])
                proj_fm(wq, QT, 4, HALO, "wq", "QT")
                proj_fm(wk, KTt, 5, 0, "wk", "KT")
                for tt in range(NTE):
                    k = pjc[0] % 2
                    pjc[0] += 1
                    for kt in range(KT):
                        MM(pj[k][:, 0:256], hT[:, kt, tt * 128:(tt + 1) * 128], wv[:, kt, :],
                           kt == 0, kt == KT - 1, ["wv"], [f"pj{k}"])
                    CP("dve", Vx[:, tt, :, 0:64], pj[k][:, 0:256].rearrange("p (h d) -> p h d", h=4),
                       [f"pj{k}"], ["Vx"])
                if g + 1 < 4:
                    LOADQKV(g + 1)
                else:
                    DMA("pool", wC[:], wblk(4096), "wC", [], ["wC"])
                emit_conv(13 if g == 0 else 9)
                items = [(h4, qt) for h4 in range(4) for qt in range(NTT)]
                n = len(items)

                def S_(i):
                    h4, qt = items[i]
                    s = i % 3
                    p0 = (h4 % 2) * 64
                    f2 = h4 // 2
                    for r in range(5):
                        o = sA3[s][:, r * 128:(r + 1) * 128] if r < 4 else sB3[s][:, 0:128]
                        MM(o, KTt[p0:p0 + 64, f2, (qt + r) * 128:(qt + r + 1) * 128],
                           QT[p0:p0 + 64, f2, qt * 128:(qt + 1) * 128], True, True,
                           ["KT", "QT"], [kA3[s] if r < 4 else kB3[s]])

                def B_(i):
                    h4, qt = items[i]
                    s = i % 3
                    STT(sb[s][:, 0:512], sA3[s][:, :], 0.125, biasS[:, h4, 0:512], ALU.mult, ALU.add,
                        [kA3[s], "bias"], [f"sb{s}a"])
                    STT(sb[s][:, 512:640], sB3[s][:, 0:128], 0.125, biasS[:, h4, 512:640], ALU.mult, ALU.add,
                        [kB3[s], "bias"], [f"sb{s}b"])

                def E_(i):
                    s = i % 3
                    ACTF(PT[s][:, :], sb[s][:, :], AF.Exp, [f"sb{s}a", f"sb{s}b"], [f"PT{s}"])

                def O_(i):
                    h4, qt = items[i]
                    s = i % 2
                    s3 = i % 3
                    for r in range(5):
                        MM(oP[s][:, 0:65], PT[s3][:, r * 128:(r + 1) * 128], Vx[:, qt + r, h4, 0:65],
                           r == 0, r == 4, [f"PT{s3}", "Vx", "Vx1"], [f"oP{s}"])

                def N_(i):
                    h4, qt = items[i]
                    s = i % 2
                    h = g * 4 + h4
                    RCP(rden[s][:, :], oP[s][:, 64:65], [f"oP{s}"], [f"rden{s}"])
                    ACTF(aout[:, qt, h * 64:(h + 1) * 64], oP[s][:, 0:64], AF.Copy, [f"oP{s}", f"rden{s}"],
                         ["aout"], scale=rden[s][:, 0:1])

                S_(0)
                S_(1)
                S_(2)
                B_(0)
                E_(0)
                B_(1)
                E_(1)
                for i in range(n):
                    if i + 2 < n:
                        B_(i + 2)
                    O_(i)
                    if i + 2 < n:
                        E_(i + 2)
                    if i + 3 < n:
                        S_(i + 3)
                    N_(i)
            P.barrier()
            P.flush()

        zc = at("zc", [128, 8, T + 2], BF, 112 * K)
        set_cur(146 * K)
        wU = loc("wU", [128, KT, 256], BF)
        wB = loc("wB", [128, KT, 256], BF)
        yb = [loc(f"yb{i}", [128, 512], F32) for i in range(2)]
        cc = [loc(f"cc{i}", [128, 512], F32) for i in range(2)]
        csq = [loc(f"csq{i}", [128, 512], BF) for i in range(2)]
        zt = loc("zt", [128, D], BF)
        junk3 = loc("junk3", [128, DA], BF)
        with ExitStack() as ph:
            pc = [ph.enter_context(nc.psum_tensor(f"pc{i}", [128, 512], F32)) for i in range(4)]
            css = ph.enter_context(nc.psum_tensor("css", [128, 512], F32))
            pcc = [0]
            first_css = [True]

            def fm_chunk(w, f2, lo, nn, wkey):
                k = pcc[0] % 4
                pcc[0] += 1
                for kt in range(KT):
                    MM(pc[k][:, 0:nn], w[:, kt, f2 * 128:(f2 + 1) * 128], hT[:, kt, lo:lo + nn],
                       kt == 0, kt == KT - 1, [wkey], [f"pc{k}"])
                return k

            def LOADW3(w, c0, j, key):
                DMA("pool", w[:], wblk(c0 + j * 256), key, [], [key])

            P.op("dve", lambda e: e.memset(zt[:], 0.0), [], ["zt"])
            for r0 in range(0, NSLOT, 1024):
                DMA("sp", Xs[r0:r0 + 1024, :].rearrange("(k p) c -> p k c", p=128),
                    zt[:, :].unsqueeze(1).to_broadcast([128, 8, D]), f"xz{(r0 // 1024) % 2}", ["zt"], [])
            DMA("sp", Ys[NSLOT:NSLOT + 128, :], zt[:, :], "yz", ["zt"], [])
            LOADW3(wU, 5120, 0, "wU")
            LOADW3(wB, 3072, 0, "wB")
            sq_todo = list(range(NTT))
            sc_todo = list(range(NTT))
            an_state = {"chain": False}

            def an_act(n):
                for _ in range(n):
                    if sq_todo:
                        tt_ = sq_todo.pop(0)
                        ACTF(junk3[:, :], aout[:, tt_, :], AF.Square, ["aout"], ["junk3", "ssa"],
                             accum_out=ssa[:, tt_:tt_ + 1])
                if not sq_todo and not an_state["chain"]:
                    an_state["chain"] = True
                    rstd_chain(ssa[:, :], None, tmp16[:, :], rstd_a[:, :], NTT, 1.0 / DA, ["ssa"], "rstd_a")

            def an_dve(n):
                if not an_state["chain"]:
                    return
                for _ in range(n):
                    if sc_todo:
                        tt_ = sc_todo.pop(0)
                        TS(aout[:, tt_, :], aout[:, tt_, :], rstd_a[:, tt_:tt_ + 1], 0.0, ALU.mult, ALU.add,
                           ["aout", "rstd_a"], ["aout"])

            for j in range(4):
                zk = lambda ft, tc: f"zc{ft}_{tc}"
                for f2 in range(2):
                    ft = 2 * j + f2
                    k = fm_chunk(wC, f2, HALO - 2, 2, "wC")
                    CP("act", zc[:, ft, 0:2], pc[k][:, 0:2], [f"pc{k}"], [zk(ft, -1)])
                    for tc in range(4):
                        k = fm_chunk(wC, f2, HALO + tc * 512, 512, "wC")
                        CP("act", zc[:, ft, 2 + tc * 512:2 + (tc + 1) * 512], pc[k][:, :], [f"pc{k}"], [zk(ft, tc)])
                        an_act(2)
                if j + 1 < 4:
                    LOADW3(wC, 4096, j + 1, "wC")
                for f2 in range(2):
                    ft = 2 * j + f2
                    k = fm_chunk(wU, f2, HALO - 2, 2, "wU")
                    TT(zc[:, ft, 0:2], pc[k][:, 0:2], zc[:, ft, 0:2], ALU.mult, [f"pc{k}", zk(ft, -1)], [zk(ft, -1)])
                    for tc in range(4):
                        k = fm_chunk(wU, f2, HALO + tc * 512, 512, "wU")
                        sl_ = zc[:, ft, 2 + tc * 512:2 + (tc + 1) * 512]
                        TT(sl_, pc[k][:, :], sl_, ALU.mult, [f"pc{k}", zk(ft, tc)], [zk(ft, tc)])
                        an_dve(2)
                if j + 1 < 4:
                    LOADW3(wU, 5120, j + 1, "wU")
                for f2 in range(2):
                    ft = 2 * j + f2
                    for tc in (3, 2, 1, 0):
                        k = fm_chunk(wB, f2, HALO + tc * 512, 512, "wB")
                        s = tc % 2
                        b0 = tc * 512
                        TS(yb[s][:, :], zc[:, ft, b0 + 2:b0 + 514], cwT[:, ft, 2:3], 0.0, ALU.mult, ALU.add,
                           [zk(ft, tc)], [f"yb{s}"])
                        STT(yb[s][:, :], zc[:, ft, b0 + 1:b0 + 513], cwT[:, ft, 1:2], yb[s][:, :], ALU.mult, ALU.add,
                            [zk(ft, tc), zk(ft, tc - 1), f"yb{s}"], [f"yb{s}"])
                        STT(yb[s][:, :], zc[:, ft, b0:b0 + 512], cwT[:, ft, 0:1], yb[s][:, :], ALU.mult, ALU.add,
                            [zk(ft, tc), zk(ft, tc - 1), f"yb{s}"], [f"yb{s}"])
                        TT(cc[s][:, :], pc[k][:, :], yb[s][:, :], ALU.mult, [f"pc{k}", f"yb{s}"], [f"cc{s}"])
                        ACTF(csq[s][:, :], cc[s][:, :], AF.Square, [f"cc{s}"], [f"csq{s}"])
                        ACTF(zc[:, ft, b0 + 2:b0 + 514], cc[s][:, :], AF.Copy, [f"cc{s}"], [zk(ft, tc)],
                             scale=gcT[:, ft:ft + 1])
                        for i in range(4):
                            tt = tc * 4 + i
                            MM(css[:, tt:tt + 1], csq[s][:, i * 128:(i + 1) * 128], ones[:, 0:1],
                               first_css[0], False, [f"csq{s}"], ["css"], skip=True)
                            first_css[0] = False
                if j + 1 < 4:
                    LOADW3(wB, 3072, j + 1, "wB")
                emit_conv(3)
            an_act(NTT)
            an_dve(NTT)
            assert not sq_todo and not sc_todo
            rstd_chain(css[:, 0:NTT], tmp16[:, :], tmp16b[:, :], rstd_c[:, :], NTT, 1.0 / DC, ["css"], "rstd_c")
            P.barrier()
            P.flush()

        aT = at("aT", [128, 8, T], BF, 0)

        Wo = at("Wo", [128, 16, D], BF, 32 * K)
        set_cur(96 * K)
        xin = [loc(f"xin5{i}", [128, D], F32) for i in range(2)]
        set_cur(146 * K)
        x2t = [loc(f"x2t{i}", [128, D], F32) for i in range(2)]
        h2 = [loc(f"h2{i}", [128, D], BF) for i in range(3)]
        g2b = loc("g2b", [128, D], F32)
        h2T = [loc(f"h2T{i}", [128, KT, 128], BF) for i in range(2)]
        junk = loc("junk5", [128, D], BF)
        wrS = loc("wrS", [128, KT, 36], BF)
        with ExitStack() as ph:
            pa = [ph.enter_context(nc.psum_tensor(f"pa{i}", [128, 512], F32)) for i in range(2)]
            pcv = [ph.enter_context(nc.psum_tensor(f"pcv{i}", [128, 512], F32)) for i in range(2)]
            tp2 = [ph.enter_context(nc.psum_tensor(f"tp2{i}", [128, 8, 128], BF)) for i in range(2)]
            pr = ph.enter_context(nc.psum_tensor("pr", [128, 512], F32))
            pr2 = ph.enter_context(nc.psum_tensor("pr2", [128, 512], F32))
            for i in range(3):
                DMA("sp", Wo[:, 4 * i:4 * i + 4, :], wob_r[:, 4 * i:4 * i + 4, :], f"wo{i}", [], [f"wo{i}"])
            DMA("pool", wrS[:], wr_d.rearrange("p (k c) -> p k c", k=KT), "wr", [], ["wr"])
            DMA("sp", g2b[:], g2b_d, "g2b", [], ["g2b"])
            for tt in range(NTT):
                s = tt % 2
                for ft in range(8):
                    TR(tp2[s][:, ft, :], aout[:, tt, ft * 128:(ft + 1) * 128], ["aout"], [f"tp2{s}"])
                TT(aT[:, :, tt * 128:(tt + 1) * 128], tp2[s][:], gaT[:, 0:8].unsqueeze(2).to_broadcast([128, 8, 128]),
                   ALU.mult, [f"tp2{s}"], ["aT"])
            DMA("sp", Wo[:, 12:16, :], wob_r[:, 12:16, :], "wo3", [], ["wo3", "aout"])
            opc = [0]

            def LOADXIN(tt):
                DMA("sp", xin[tt % 2][:], x_ext[HALO + tt * 128:HALO + (tt + 1) * 128, :], f"xin{tt % 2}", [],
                    [f"xin{tt % 2}"] + (["aout"] if tt < 2 else []))

            LOADXIN(0)

            def OUTPROJ(tt, hooks=None):
                s = tt % 2
                hooks = hooks or {}
                if tt + 1 < NTT:
                    LOADXIN(tt + 1)
                for n4 in range(4):
                    if n4 > 0 and (n4 - 1) in hooks:
                        hooks[n4 - 1]()
                    k = opc[0] % 2
                    opc[0] += 1
                    cs = slice(n4 * 512, (n4 + 1) * 512)
                    for ft in range(8):
                        MM(pa[k][:, :], aT[:, ft, tt * 128:(tt + 1) * 128], Wo[:, ft, cs], ft == 0, ft == 7,
                           [f"wo{ft // 4}", "aT"], [f"pa{k}"])
                    for ft in range(8):
                        MM(pcv[k][:, :], zc[:, ft, 2 + tt * 128:2 + (tt + 1) * 128], Wo[:, 8 + ft, cs],
                           ft == 0, ft == 7, [f"wo{2 + ft // 4}"], [f"pcv{k}"])
                    STT(x2t[s][:, cs], pcv[k][:, :], rstd_c[:, tt:tt + 1], xin[s][:, cs], ALU.mult, ALU.add,
                        [f"pcv{k}", f"xin{s}"], [f"x2t{s}_{n4}"])
                    TT(x2t[s][:, cs], x2t[s][:, cs], pa[k][:, :], ALU.add, [f"pa{k}", f"x2t{s}_{n4}"],
                       [f"x2t{s}_{n4}"])
                x2k = [f"x2t{s}_{n4}" for n4 in range(4)]
                DMA("sp", x2s[tt * 128:(tt + 1) * 128, :], x2t[s][:], f"x2o{s}", x2k, [])
                ACTF(junk[:], x2t[s][:], AF.Square, x2k, ["junk", f"ss{s}"], accum_out=st[s][:, 0:1])
                rstd_chain(st[s][:, 0:1], st[s][:, 1:2], st[s][:, 2:3], st[s][:, 3:4], 1, 1.0 / D,
                           [f"ss{s}"], f"rs{s}")
                STT(h2[tt % 3][:], x2t[s][:], st[s][:, 3:4], g2b[:], ALU.mult, ALU.mult, x2k + [f"rs{s}", "g2b"],
                    [f"h2_{tt % 3}"])

            def TRA(tt):
                s = tt % 2
                s3 = tt % 3
                for h in range(2):
                    for j in range(8):
                        kt = h * 8 + j
                        TR(tp2[h][:, j, :], h2[s3][:, kt * 128:(kt + 1) * 128], [f"h2_{s3}"], [f"tp2{h}"])
                    CP("act", h2T[s][:, h * 8:(h + 1) * 8, :], tp2[h][:], [f"tp2{h}"], [f"h2T{s}{h}"])

            def RA(tt):
                s = tt % 2
                for kt in range(KT):
                    MM(pr[:, 0:36], h2T[s][:, kt, :], wrS[:, kt, :], kt == 0, kt == KT - 1,
                       [f"h2T{s}0", f"h2T{s}1", "wr"], ["pr"])
                TT(lg[:, :], pr[:, 0:36], brb[:, :], ALU.add, ["pr"], ["lg"])
                RED(gmx[:, 0:1], lg[:, 0:4], ALU.max, ["lg"], ["gmax"])
                TT(ohg[:, :], lg[:, 0:4], gmx[:, 0:1].to_broadcast([128, 4]), ALU.is_equal, ["lg", "gmax"], ["ohg"])
                TS(gmx[:, 1:2], gmx[:, 0:1], -1.0, 0.0, ALU.mult, ALU.add, ["gmax"], ["ngmax"])
                ACTF(eg[:, :], lg[:, 0:4], AF.Exp, ["lg", "ngmax"], ["eg", "gsum"], bias=gmx[:, 1:2],
                     accum_out=gmx[:, 2:3])
                TS(pen[:, :], ohg[:, :], 30000.0, -30000.0, ALU.mult, ALU.add, ["ohg"], ["pen"])
                TT(elm[:, :].rearrange("p (g e) -> p g e", g=4), lg[:, 4:36].rearrange("p (g e) -> p g e", g=4),
                   pen[:, 0:4].unsqueeze(2).to_broadcast([128, 4, 8]), ALU.add, ["lg", "pen"], ["elm"])
                P.op("dve", lambda e: e.max(out=m8[:, :], in_=elm[:, :]), ["elm"], ["m8"])
                TT(oh[:, :, :], elm[:, :].unsqueeze(1).to_broadcast([128, 2, NE]),
                   m8[:, 0:2].unsqueeze(2).to_broadcast([128, 2, NE]), ALU.is_equal, ["elm", "m8"], ["oh"])
                TT(Mb[:, :], oh[:, 0, :], oh[:, 1, :], ALU.add, ["oh"], ["Mb"])
                TT(gsc[:, 0:1], m8[:, 1:2], m8[:, 0:1], ALU.subtract, ["m8"], ["gd"])
                ACTF(gsc[:, 1:2], gsc[:, 0:1], AF.Exp, ["gd"], ["ged"])
                TS(gsc[:, 2:3], gsc[:, 1:2], 1.0, 0.0, ALU.add, ALU.add, ["ged"], ["gden"])
                TT(gsc[:, 3:4], gsc[:, 2:3], gmx[:, 2:3], ALU.mult, ["gden", "gsum"], ["gdg"])
                RCP(gw[:, tt, 0:1], gsc[:, 3:4], ["gdg"], [f"gw{tt}a"])
                TT(gw[:, tt, 1:2], gw[:, tt, 0:1], gsc[:, 1:2], ALU.mult, [f"gw{tt}a", "ged"], [f"gw{tt}b"])

            def RB(tt):
                s3 = tt % 3
                MM(pr2[:, 64:96], tri[:, :], Mb[:, :], True, True, ["Mb"], ["pr2"])
                MM(pr2[:, 128:160], ones[:, :], Mb[:, :], True, True, ["Mb"], ["pr2"])
                TT(rk[:, :], pr2[:, 64:96], cnt[:, :], ALU.add, ["pr2", "cnt"], ["rk"])
                TT(cnt[:, :], cnt[:, :], pr2[:, 128:160], ALU.add, ["pr2", "cnt"], ["cnt"])
                TT(tmpe[:, :], rk[:, :], ecap[:, :], ALU.add, ["rk"], ["tmpe"])
                TT(prod[:, :, :], oh[:, :, :], tmpe[:, :].unsqueeze(1).to_broadcast([128, 2, NE]), ALU.mult,
                   ["oh", "tmpe"], ["prod"])
                RED(slf[:, :], prod[:, :, :], ALU.add, ["prod"], ["slf"])
                TT(prod[:, :, :], oh[:, :, :], rk[:, :].unsqueeze(1).to_broadcast([128, 2, NE]), ALU.mult,
                   ["oh", "rk", "slf"], ["prod"])
                RED(rr[:, :], prod[:, :, :], ALU.add, ["prod"], ["rr"])
                TS(tov[:, :], rr[:, :], -float(CAP - 1), 0.0, ALU.add, ALU.max, ["rr"], ["tov"])
                TS(tov[:, :], tov[:, :], 1.0, 1.0, ALU.min, ALU.mult, ["tov"], ["tov"])
                TS(dsl[:, :], slf[:, :], -1.0, float(NSLOT), ALU.mult, ALU.add, ["slf"], ["dsl"])
                TT(dsl[:, :], dsl[:, :], tov[:, :], ALU.mult, ["dsl", "tov"], ["dsl"])
                TT(slf[:, :], slf[:, :], dsl[:, :], ALU.add, ["slf", "dsl"], ["slf"])
                CP("dve", sl_i[:, tt, :], slf[:, :], ["slf"], [f"sl{tt}"])
                if 1 <= tt <= 9:
                    emit_conv(1)
                for kk in range(2):
                    P.dma("pool", lambda e, kk=kk, s3=s3, tt=tt: e.indirect_dma_start(
                        out=Xs[:, :], out_offset=bass.IndirectOffsetOnAxis(ap=sl_i[:, tt, kk:kk + 1], axis=0),
                        in_=h2[s3][:, :], in_offset=None, bounds_check=NSLOT - 1, oob_is_err=False),
                        f"sc{s3}{kk}", [f"h2_{s3}", f"sl{tt}"], [])

            OUTPROJ(0)
            OUTPROJ(1, hooks={1: (lambda: TRA(0)), 2: (lambda: RA(0))})
            for tt in range(NTT):
                hk = {0: (lambda t=tt: RB(t))}
                if tt + 1 < NTT:
                    hk[1] = (lambda t=tt + 1: TRA(t))
                    hk[2] = (lambda t=tt + 1: RA(t))
                if tt + 2 < NTT:
                    OUTPROJ(tt + 2, hooks=hk)
                else:
                    for kk_ in (0, 1, 2):
                        if kk_ in hk:
                            hk[kk_]()
            set_cur(0)
            wg = [loc(f"wg{i}", [128, KT, DE], BF) for i in range(2)]
            wu = [loc(f"wu{i}", [128, KT, DE], BF) for i in range(2)]
            wd = [loc(f"wd{i}", [128, 4, D], BF) for i in range(2)]
            pk_ = ["aT", "wo0", "wo1", "wo2", "wo3"]
            DMA("sp", wg[0][:], wgb[0].rearrange("p (k c) -> p k c", k=KT), "wg0", [], ["wg0"] + pk_)
            DMA("sp", wu[0][:], wub[0].rearrange("p (k c) -> p k c", k=KT), "wu0", [], ["wu0"] + pk_)
            DMA("sp", wd[0][:], wdb[0].rearrange("p (j c) -> p j c", j=4), "wd0", [], ["wd0"] + pk_)
            DMA("sp", wg[1][:], wgb[1].rearrange("p (k c) -> p k c", k=KT), "wg1", [], ["wg1"] + pk_)
            DMA("sp", wu[1][:], wub[1].rearrange("p (k c) -> p k c", k=KT), "wu1", [], ["wu1"] + pk_)
            DMA("sp", wd[1][:], wdb[1].rearrange("p (j c) -> p j c", j=4), "wd1", [], ["wd1"] + pk_)
            if dbg is not None:
                P.op("dve", lambda e: e.tensor_copy(out=x2t[0][:, 0:NTT * 8].rearrange("p (t k) -> p t k", k=8)[:, :, 0:2],
                                                    in_=sl_i[:, :, :]), [], ["dbgt"] + [f"x2t0_{n4}" for n4 in range(4)])
                P.op("dve", lambda e: e.tensor_copy(out=x2t[0][:, 0:NTT * 8].rearrange("p (t k) -> p t k", k=8)[:, :, 2:4],
                                                    in_=gw[:, :, :]), ["dbgt"], ["dbgt"])
                DMA("sp", dbg, x2t[0][:, 0:NTT * 8].rearrange("p (t k) -> p t k", k=8), "dbg", ["dbgt"], [])
            P.barrier()
            P.flush()

        if stage >= 6:
            set_cur(96 * K)
            xr = [loc(f"xr{i}", [128, NSUB, D], BF) for i in range(2)]
            xbT = [loc(f"xbT{i}", [128, KT, CAP], BF) for i in range(2)]
            aTe = [loc(f"aTe{i}", [128, 4, CAP], BF) for i in range(2)]
            sg = [loc(f"sg{i}", [128, CAP], F32) for i in range(2)]
            ysb = [loc(f"ysb{i}", [128, D], BF) for i in range(4)]
            with ExitStack() as ph:
                tpe = [ph.enter_context(nc.psum_tensor(f"tpe{i}", [128, 8, 128], BF)) for i in range(2)]
                pg_ = [ph.enter_context(nc.psum_tensor(f"pg{i}", [128, 512], F32)) for i in range(2)]
                pu_ = [ph.enter_context(nc.psum_tensor(f"pu{i}", [128, 512], F32)) for i in range(2)]
                py = [ph.enter_context(nc.psum_tensor(f"py{i}", [128, 512], F32)) for i in range(2)]
                c1 = [0, 0, 0, 0]

                def LOADW(e_):
                    s = e_ % 2
                    if e_ < NCONV:
                        DMA("sp", wg[s][:], wgb[e_].rearrange("p (k c) -> p k c", k=KT), f"wg{s}", [], [f"wg{s}"])
                        DMA("sp", wu[s][:], wub[e_].rearrange("p (k c) -> p k c", k=KT), f"wu{s}", [], [f"wu{s}"])
                        DMA("sp", wd[s][:], wdb[e_].rearrange("p (j c) -> p j c", j=4), f"wd{s}", [], [f"wd{s}"])
                    else:
                        DMA("pool", wg[s][:], w_gate[e_].rearrange("p (k c) -> p k c", k=KT), f"wgp{s}", [], [f"wg{s}"])
                        DMA("pool", wu[s][:], w_up[e_].rearrange("p (k c) -> p k c", k=KT), f"wup{s}", [], [f"wu{s}"])
                        DMA("pool", wd[s][:], w_down[e_].rearrange("p (j c) -> p j c", j=4), f"wdp{s}", [], [f"wd{s}"])

                def LOADX(e_):
                    s = e_ % 2
                    nf = CAP // 128
                    DMA("sp", xr[s][:, 0:nf, :],
                        Xs[e_ * CAP:e_ * CAP + nf * 128, :].rearrange("(sub p) c -> p sub c", p=128),
                        f"xr{s}", [], [f"xr{s}a"])
                    if CAP % 128:
                        rem = CAP % 128
                        DMA("sp", xr[s][0:rem, nf, :], Xs[e_ * CAP + nf * 128:(e_ + 1) * CAP, :],
                            f"xr{s}b", [], [f"xr{s}b"])

                def TRX(e_):
                    s = e_ % 2
                    for sub in range(NSUB):
                        rs = RSZ[sub]
                        xkey = f"xr{s}a" if rs == 128 else f"xr{s}b"
                        for h in range(2):
                            k = c1[0] % 2
                            c1[0] += 1
                            for j in range(8):
                                kt = h * 8 + j
                                P.op("pe", lambda e, k=k, j=j, s=s, sub=sub, kt=kt, rs=rs: e.transpose(
                                    out=tpe[k][:, j, 0:rs], in_=xr[s][0:rs, sub, kt * 128:(kt + 1) * 128],
                                    identity=ident[0:rs, 0:rs]), [xkey], [f"tpe{k}"])
                            CP("act" if k == 0 else "dve", xbT[s][:, h * 8:(h + 1) * 8, sub * 128:sub * 128 + rs],
                               tpe[k][:, :, 0:rs], [f"tpe{k}"], [f"xbT{s}_{sub}{h}"])

                def GU(e_):
                    s = e_ % 2
                    xk = [f"xbT{s}_{a}{b}" for a in range(NSUB) for b in range(2)]
                    for jt in range(4):
                        k = c1[1] % 2
                        c1[1] += 1
                        for kt in range(KT):
                            MM(pg_[k][:, 0:CAP], wg[s][:, kt, jt * 128:(jt + 1) * 128], xbT[s][:, kt, :],
                               kt == 0, kt == KT - 1, xk + [f"wg{s}"], [f"pg{k}"])
                        for kt in range(KT):
                            MM(pu_[k][:, 0:CAP], wu[s][:, kt, jt * 128:(jt + 1) * 128], xbT[s][:, kt, :],
                               kt == 0, kt == KT - 1, xk + [f"wu{s}"], [f"pu{k}"])
                        ACTF(sg[k][:, :], pg_[k][:, 0:CAP], AF.Silu, [f"pg{k}"], [f"sg{k}"])
                        TT(aTe[s][:, jt, :], sg[k][:, :], pu_[k][:, 0:CAP], ALU.mult, [f"pu{k}", f"sg{k}"],
                           [f"aTe{s}_{jt}"])

                def YY(e_):
                    s = e_ % 2
                    ak = [f"aTe{s}_{jt}" for jt in range(4)]
                    for sub in range(NSUB):
                        rs = RSZ[sub]
                        ys = c1[2] % 4
                        c1[2] += 1
                        for n4 in range(4):
                            k = c1[3] % 2
                            c1[3] += 1
                            for jt in range(4):
                                MM(py[k][0:rs, :], aTe[s][:, jt, sub * 128:sub * 128 + rs],
                                   wd[s][:, jt, n4 * 512:(n4 + 1) * 512], jt == 0, jt == 3,
                                   ak + [f"wd{s}"], [f"py{k}"])
                            CP("act" if n4 % 2 == 0 else "dve", ysb[ys][0:rs, n4 * 512:(n4 + 1) * 512], py[k][0:rs, :],
                               [f"py{k}"], [f"ysb{ys}_{n4}"])
                        r0 = e_ * CAP + sub * 128
                        DMA("pool", Ys[r0:r0 + rs, :], ysb[ys][0:rs, :], f"yo{ys}", [f"ysb{ys}_{n4}" for n4 in range(4)], [])

                LOADX(0)
                TRX(0)
                for e_ in range(NE):
                    if e_ + 1 < NE:
                        if e_ + 1 != 1:
                            LOADW(e_ + 1)
                        LOADX(e_ + 1)
                    GU(e_)
                    if e_ + 1 < NE:
                        TRX(e_ + 1)
                    YY(e_)
                    if e_ == NE - 4:
                        set_cur(165 * K)
                        x2t = [loc(f"x2f{i}", [128, D], F32) for i in range(3)]
                        gfb = loc("gfb", [128, D], F32)
                        DMA("sp", gfb[:], gfb_d, "gfb", [], ["gfb"])
                        for t7 in range(3):
                            DMA("sp", x2t[t7][:], x2s[t7 * 128:(t7 + 1) * 128, :], f"x2f{t7}", [], [f"x2f{t7}"])
                P.barrier()
                P.flush()

            set_cur(0)
            ya = [loc(f"ya{i}", [128, D], BF) for i in range(3)]
            ybb = [loc(f"ybb{i}", [128, D], BF) for i in range(3)]
            x3 = [loc(f"x3{i}", [128, D], F32) for i in range(2)]
            ot = [loc(f"ot{i}", [128, D], F32) for i in range(2)]
            junk = loc("junk7", [128, D], BF)

            def LOAD7(tt):
                s3 = tt % 3
                if tt >= 3:
                    DMA("sp", x2t[s3][:], x2s[tt * 128:(tt + 1) * 128, :], f"x2f{s3}", [], [f"x2f{s3}"])
                for kk, dst in ((0, ya), (1, ybb)):
                    P.dma("pool", lambda e, kk=kk, dst=dst, s3=s3, tt=tt: e.indirect_dma_start(
                        out=dst[s3][:, :], out_offset=None, in_=Ys[:, :],
                        in_offset=bass.IndirectOffsetOnAxis(ap=sl_i[:, tt, kk:kk + 1], axis=0)),
                        f"yg{s3}{kk}", [], [f"yg{s3}{kk}"])

            def X37(tt):
                s = tt % 2
                s3 = tt % 3
                STT(x3[s][:], ya[s3][:], gw[:, tt, 0:1], x2t[s3][:], ALU.mult, ALU.add, [f"yg{s3}0", f"x2f{s3}"],
                    [f"x3{s}"])
                STT(x3[s][:], ybb[s3][:], gw[:, tt, 1:2], x3[s][:], ALU.mult, ALU.add, [f"yg{s3}1", f"x3{s}"], [f"x3{s}"])
                ACTF(junk[:], x3[s][:], AF.Square, [f"x3{s}"], ["junk", f"ss{s}"], accum_out=st[s][:, 0:1])
                ACTF(st[s][:, 2:3], st[s][:, 0:1], AF.Sqrt, [f"ss{s}"], [f"rs{s}_s"], scale=1.0 / D, bias=epsT[:, 0:1])

            def FIN7(tt):
                s = tt % 2
                RCP(st[s][:, 3:4], st[s][:, 2:3], [f"rs{s}_s"], [f"rs{s}"])
                STT(ot[s][:], x3[s][:], st[s][:, 3:4], gfb[:], ALU.mult, ALU.mult, [f"x3{s}", f"rs{s}", "gfb"], [f"ot{s}"])
                DMA("sp", out_d[tt * 128:(tt + 1) * 128, :], ot[s][:], f"oo{s}", [f"ot{s}"], [])

            LOAD7(0)
            LOAD7(1)
            X37(0)
            for tt in range(NTT):
                if tt + 2 < NTT:
                    LOAD7(tt + 2)
                if tt + 1 < NTT:
                    X37(tt + 1)
                FIN7(tt)
            P.barrier()
            P.flush()
    return nc


def _bias_table(rel_bias):
    kk = np.arange(128)[:, None, None]
    r = np.arange(5)[None, :, None]
    qq = np.arange(128)[None, None, :]
    dist = 512 + qq - r * 128 - kk
    idx = np.clip(dist, -256, 256) + 256
    dch = (8 + qq // 64) - (2 * r + kk // 64)
    ok = (dch >= 0) & (dch <= 8)
    tab = rel_bias[:, idx]
    tab = np.where(ok[None], tab, np.float32(-1e30)).astype(np.float32)
    return np.ascontiguousarray(tab.transpose(1, 0, 2, 3).reshape(128, 16, 640))


def kernel(x, norm1, w_in, rel_bias, conv_w, g_out_attn, g_out_conv, w_out, norm2,
           w_router_group, b_router_group, w_router_expert, b_router_expert,
           w_gate, w_up, w_down, norm_final):
    stage = int(os.environ.get("MK_STAGE", "99"))
    f = lambda a: np.ascontiguousarray(np.asarray(a, dtype=np.float32))
    x = f(x)
    B, S, _ = x.shape
    shared = dict(
        w_in=f(np.asarray(w_in[0]).reshape(KT, 128, 24, 256).transpose(2, 1, 0, 3)).reshape(24, 128, KT * 256),
        g1T=f(np.asarray(norm1[0]).reshape(KT, 128).T),
        biasT=_bias_table(f(rel_bias[0])),
        cwT=f(np.asarray(conv_w[0]).reshape(3, 8, 128).transpose(2, 1, 0)),
        gaT=f(np.asarray(g_out_attn[0]).reshape(8, 128).T),
        gcT=f(np.asarray(g_out_conv[0]).reshape(8, 128).T),
        w_out=f(np.asarray(w_out[0]).reshape(KT, 128, D).transpose(1, 0, 2)).reshape(128, KT * D),
        g2b=f(np.broadcast_to(np.asarray(norm2[0])[None, :], (128, D))),
        gfb=f(np.broadcast_to(np.asarray(norm_final)[None, :], (128, D))),
        wr=f(np.concatenate([np.asarray(w_router_group[0]), np.asarray(w_router_expert[0])], axis=1)
             .reshape(KT, 128, 36).transpose(1, 0, 2)).reshape(128, KT * 36),
        brb=f(np.broadcast_to(np.concatenate([np.asarray(b_router_group[0]),
                                              np.asarray(b_router_expert[0])])[None, :], (128, 36))),
        w_gate=f(np.asarray(w_gate[0]).reshape(NE, KT, 128, DE).transpose(0, 2, 1, 3)).reshape(NE, 128, KT * DE),
        w_up=f(np.asarray(w_up[0]).reshape(NE, KT, 128, DE).transpose(0, 2, 1, 3)).reshape(NE, 128, KT * DE),
        w_down=f(np.asarray(w_down[0]).reshape(NE, 4, 128, D).transpose(0, 2, 1, 3)).reshape(NE, 128, 4 * D),
        ident=np.eye(128, dtype=np.float32).astype(bf16),
        tri=np.triu(np.ones((128, 128), np.float32), 1).astype(bf16),
        ones=np.ones((128, 128), np.float32).astype(bf16),
        ecap=f(np.broadcast_to((np.arange(NE) * CAP)[None, :], (128, NE))),
    )
    in_maps = []
    for c in range(NCORES):
        b, half = c // 2, c % 2
        xe = np.zeros((TE, D), np.float32)
        if half == 1:
            xe[:HALO] = x[b, T - HALO:T]
        xe[HALO:] = x[b, half * T:(half + 1) * T]
        val = np.ones((NTE, 128), np.float32)
        if half == 0:
            val[:HALO // 128] = 0.0
        m = dict(shared)
        m["x_ext"] = xe
        m["valid"] = np.ascontiguousarray(val.T)
        in_maps.append(m)
    nc = build(stage)
    res = run_bass_kernel_spmd(nc, in_maps, core_ids=list(range(NCORES)))
    key = "out" if stage >= 99 else "x2s"
    out = np.empty((B, S, D), np.float32)
    for c in range(NCORES):
        b, half = c // 2, c % 2
        out[b, half * T:(half + 1) * T] = res.results[c][key]
    if stage < 99:
        kernel.dbg = [res.results[c]["dbg"] for c in range(NCORES)]
    return out
```
